# Optimizing a Trainium2 kernel written in Bass

```python
import math
import jax, jax.numpy as jnp
from jax import lax
import numpy as np


D_MODEL = 1024
BATCH = 2
SEQ = 8192
DEPTH = 2

PLE_DIM = 256
N_EVEN = (DEPTH + 1) // 2
N_ODD = DEPTH // 2

H_A = 8
D_NOPE = 64
D_VA = 64
R_Q = 256
R_KV = 128
H_IDX = 8
D_IDX = 64
TOPK_MAX = 256
Q_BLOCK = 128

H_B = 4
D_B = 128
CONV_K = 4
DN_CHUNK = 64

H_C = 12
DH_C = 64
DILATED_PATTERNS = ((128, 1), (512, 4), (2048, 16))

POOL_WINDOWS = (2, 4, 8, 16)
POOL_GROUP = 64

N_EXPERTS = 32
TOP_K = 4
D_EXPERT = 1024
SWIGLU_LIMIT = 7.0
SWIGLU_ALPHA = 1.702
EXPERT_BLOCK = 128

ALPHA = (2 * DEPTH) ** 0.25
BETA = (8 * DEPTH) ** -0.25

AB_SPLITS = (R_Q, R_KV, D_IDX, H_IDX, H_B * D_B, H_B * D_B, H_B * D_B, H_B * D_B, H_B, H_B)
CD_SPLITS = (H_C * DH_C, H_C * DH_C, H_C * DH_C, len(POOL_WINDOWS) * POOL_GROUP)
W_AB = H_A * D_VA + H_B * D_B
W_CD = H_C * DH_C + len(POOL_WINDOWS) * POOL_GROUP

kernel_name = "hybrid_dsa_gdn_dilated_pool_moe"

F32 = jnp.float32


def _split(t, sizes):
    outs, start = [], 0
    for n in sizes:
        outs.append(t[..., start:start + n])
        start += n
    return outs


def _layernorm(x, g, b, eps=1e-5):
    xf = x.astype(F32)
    mu = xf.mean(-1, keepdims=True)
    var = jnp.square(xf - mu).mean(-1, keepdims=True)
    return ((xf - mu) * lax.rsqrt(var + eps) * g + b).astype(x.dtype)


def _rmsnorm(x, g, eps=1e-6):
    xf = x.astype(F32)
    return (xf * lax.rsqrt(jnp.mean(xf * xf, -1, keepdims=True) + eps) * g).astype(x.dtype)


def _l2norm(x, eps=1e-6):
    return x * lax.rsqrt(jnp.sum(x * x, -1, keepdims=True) + eps)


def _dsa_attention(c_q, c_kv, k_idx, w_idx, w_uq, w_uk, w_uv, w_qidx):
    B, S, _ = c_q.shape
    topk = min(TOPK_MAX, S // 4)
    nb = S // Q_BLOCK
    q = jnp.einsum('bsr,rhd->bshd', c_q, w_uq)
    q_lat = jnp.einsum('bshd,rhd->bshr', q, w_uk) * D_NOPE ** -0.5
    q_idx = jnp.einsum('bsr,rhd->bshd', c_q, w_qidx)
    w_idx = w_idx * (H_IDX ** -0.5 * D_IDX ** -0.5)
    key_pos = jnp.arange(S)
    q_pos = key_pos.reshape(nb, Q_BLOCK)

    def blocks(t):
        return jnp.moveaxis(t.reshape(B, nb, Q_BLOCK, *t.shape[2:]), 1, 0)

    def attend(args):
        qi_idx, wi, qi_lat, pos = args
        sc = jnp.einsum('bqhd,bkd->bqhk', qi_idx, k_idx)
        isc = jnp.einsum('bqhk,bqh->bqk', jax.nn.relu(sc), wi).astype(F32)
        causal = key_pos[None, :] <= pos[:, None]
        isc = jnp.where(causal[None], isc, -jnp.inf)
        _, sel = lax.top_k(isc, topk)
        kv_sel = jax.vmap(lambda c, i: c[i])(c_kv, sel)
        s = jnp.einsum('bqhr,bqkr->bqhk', qi_lat, kv_sel).astype(F32)
        valid = sel <= pos[None, :, None]
        s = jnp.where(valid[:, :, None, :], s, -jnp.inf)
        pr = jax.nn.softmax(s, axis=-1).astype(kv_sel.dtype)
        return jnp.einsum('bqhk,bqkr->bqhr', pr, kv_sel)

    o_lat = lax.map(attend, (blocks(q_idx), blocks(w_idx), blocks(q_lat), q_pos))
    o_lat = jnp.moveaxis(o_lat, 0, 1).reshape(B, S, H_A, R_KV)
    o = jnp.einsum('bshr,rhv->bshv', o_lat, w_uv)
    return o.reshape(B, S, H_A * D_VA)


def _causal_conv(x, w):
    C = x.shape[-1]
    K = w.shape[0]
    return lax.conv_general_dilated(x, w[:, None, :], window_strides=(1,), padding=[(K - 1, 0)],
                                    dimension_numbers=('NWC', 'WIO', 'NWC'), feature_group_count=C)


def _chunk_gated_delta(q, k, v, g, beta):
    B, S, H, Dk = q.shape
    Dv = v.shape[-1]
    C = DN_CHUNK
    n = S // C

    def chunks(t):
        return t.reshape(B, n, C, H, -1).transpose(1, 0, 3, 2, 4)

    q, k, v = chunks(q), chunks(k), chunks(v)
    g = g.reshape(B, n, C, H).transpose(1, 0, 3, 2)
    beta = beta.reshape(B, n, C, H).transpose(1, 0, 3, 2)
    gc = jnp.cumsum(g, axis=-1)
    lower = jnp.tril(jnp.ones((C, C), bool))
    strict = jnp.tril(jnp.ones((C, C), bool), -1)
    decay = jnp.exp(jnp.where(lower, gc[..., :, None] - gc[..., None, :], -jnp.inf))
    kb = k * beta[..., None]
    vb = v * beta[..., None]
    a_mat = jnp.where(strict, jnp.einsum('...id,...jd->...ij', kb, k) * decay, 0.0)
    eye = jnp.eye(C, dtype=F32)
    t_mat = lax.linalg.triangular_solve(a_mat + eye, jnp.broadcast_to(eye, a_mat.shape),
                                        left_side=True, lower=True, unit_diagonal=True)
    u = t_mat @ vb
    w = t_mat @ (kb * jnp.exp(gc)[..., None])
    qk = jnp.where(lower, jnp.einsum('...id,...jd->...ij', q, k) * decay, 0.0)
    q_dec = q * jnp.exp(gc)[..., None]
    k_dec = k * jnp.exp(gc[..., -1:] - gc)[..., None]
    g_last = jnp.exp(gc[..., -1])

    def step(state, xs):
        q_i, k_i, u_i, w_i, qk_i, gl_i = xs
        v_new = u_i - w_i @ state
        o_i = q_i @ state + qk_i @ v_new
        state = state * gl_i[..., None, None] + jnp.einsum('bhck,bhcv->bhkv', k_i, v_new)
        return state, o_i

    state0 = jnp.zeros((B, H, Dk, Dv), F32)
    _, o = lax.scan(step, state0, (q_dec, k_dec, u, w, qk, g_last))
    return o.transpose(1, 0, 3, 2, 4).reshape(B, S, H, Dv)


def _gated_deltanet(q, k, v, z, b, a, conv_w, a_log, dt_bias, norm_g):
    B, S, _ = q.shape
    W = H_B * D_B
    qkv = jax.nn.silu(_causal_conv(jnp.concatenate([q, k, v], -1), conv_w))
    q, k, v = [t.reshape(B, S, H_B, D_B).astype(F32) for t in _split(qkv, (W, W, W))]
    q = _l2norm(q) * D_B ** -0.5
    k = _l2norm(k)
    beta = jax.nn.sigmoid(b.astype(F32))
    g = -jnp.exp(a_log.astype(F32)) * jax.nn.softplus(a.astype(F32) + dt_bias.astype(F32))
    o = _chunk_gated_delta(q, k, v, g, beta)
    o = _rmsnorm(o, norm_g) * jax.nn.silu(z.reshape(B, S, H_B, D_B).astype(F32))
    return o.reshape(B, S, W).astype(z.dtype)


def _mixer_ab(x, w_in, q_norm_g, kv_norm_g, w_uq, w_uk, w_uv, w_qidx, kidx_norm_g, kidx_norm_b,
              conv_w, a_log, dt_bias, out_norm_g, w_out):
    c_q, c_kv, k_idx, w_idx, qb, kb, vb, zb, bb, ab = _split(x @ w_in, AB_SPLITS)
    c_q = _rmsnorm(c_q, q_norm_g)
    c_kv = _rmsnorm(c_kv, kv_norm_g)
    k_idx = _layernorm(k_idx, kidx_norm_g, kidx_norm_b)
    o_a = _dsa_attention(c_q, c_kv, k_idx, w_idx, w_uq, w_uk, w_uv, w_qidx)
    o_b = _gated_deltanet(qb, kb, vb, zb, bb, ab, conv_w, a_log, dt_bias, out_norm_g)
    return jnp.concatenate([o_a.astype(x.dtype), o_b.astype(x.dtype)], -1) @ w_out


def _dilated_branch(q, k, v, window, dilation):
    B, S, H, Dh = q.shape
    R = window // dilation
    seg = dilation * R
    S_pad = -(-S // seg) * seg
    N = S_pad // dilation
    nb = N // R

    def to_blocks(t):
        t = jnp.pad(t, ((0, 0), (0, S_pad - S), (0, 0), (0, 0)))
        t = t.reshape(B, N, dilation, H, Dh).transpose(0, 2, 1, 3, 4)
        return t.reshape(B, dilation, nb, R, H, Dh)

    def with_prev(t):
        prev = jnp.pad(t, ((0, 0), (0, 0), (1, 0), (0, 0), (0, 0), (0, 0)))[:, :, :-1]
        return jnp.concatenate([prev, t], axis=3)

    qb = to_blocks(q)
    k2 = with_prev(to_blocks(k))
    v2 = with_prev(to_blocks(v))
    s = jnp.einsum('bcnqhd,bcnkhd->bcnqhk', qb, k2).astype(F32) * Dh ** -0.5
    r = jnp.arange(R)[:, None]
    j = jnp.arange(2 * R)[None, :]
    dist = R + r - j
    band = (dist >= 0) & (dist <= R)
    has_prev = (jnp.arange(nb) > 0)[:, None, None] | (j >= R)[None]
    mask = band[None] & has_prev
    s = jnp.where(mask[:, :, None, :], s, -jnp.inf)
    m = s.max(-1)
    e = jnp.exp(s - m[..., None])
    den = e.sum(-1)
    o = jnp.einsum('bcnqhk,bcnkhd->bcnqhd', e, v2.astype(F32)) / den[..., None]

    def from_blocks(t):
        t = t.reshape(B, dilation, N, *t.shape[4:])
        t = jnp.moveaxis(t, 1, 2)
        return t.reshape(B, S_pad, *t.shape[3:])[:, :S]

    return from_blocks(o), from_blocks(m), from_blocks(den)


def _dilated_attention(q, k, v):
    res = [_dilated_branch(q, k, v, w, d) for (w, d) in DILATED_PATTERNS]
    o = jnp.stack([t[0] for t in res])
    m = jnp.stack([t[1] for t in res])
    den = jnp.stack([t[2] for t in res])
    wgt = den * jnp.exp(m - m.max(0))
    return (wgt[..., None] * o).sum(0) / wgt.sum(0)[..., None]


def _multiscale_pool(u, pool_w, pool_scale):
    B, S, _ = u.shape
    count = jnp.arange(1, S + 1).astype(F32)
    outs = []
    for gi, w in enumerate(POOL_WINDOWS):
        xg = u[..., gi * POOL_GROUP:(gi + 1) * POOL_GROUP].astype(F32)
        c = jnp.cumsum(xg, axis=1)
        c_prev = jnp.pad(c, ((0, 0), (w, 0), (0, 0)))[:, :S]
        mean = (c - c_prev) / jnp.minimum(count, w)[None, :, None]
        outs.append((mean - xg) @ pool_w[gi].astype(F32))
    return (jnp.concatenate(outs, -1) * pool_scale).astype(u.dtype)


def _mixer_cd(x, w_in, pool_w, pool_scale, w_out):
    B, S, _ = x.shape
    qc, kc, vc, ud = _split(x @ w_in, CD_SPLITS)
    shp = (B, S, H_C, DH_C)
    o_c = _dilated_attention(qc.reshape(shp), kc.reshape(shp), vc.reshape(shp))
    o_d = _multiscale_pool(ud, pool_w, pool_scale)
    return jnp.concatenate([o_c.reshape(B, S, H_C * DH_C).astype(x.dtype), o_d], -1) @ w_out


def _moe(x, router_w, router_b, w_gu, b_gu, w_down, b_down):
    B, S, D = x.shape
    xt = x.reshape(-1, D)
    T = xt.shape[0]
    logits = (xt @ router_w + router_b).astype(F32)
    top_logit, top_e = lax.top_k(logits, TOP_K)
    gate = jax.nn.softmax(top_logit, axis=-1)
    TK = T * TOP_K
    flat_e = top_e.reshape(-1).astype(jnp.int32)
    flat_tok = jnp.arange(TK, dtype=jnp.int32) // TOP_K
    flat_gate = gate.reshape(-1)
    order = jnp.argsort(flat_e)
    se = flat_e[order]
    counts = jnp.zeros((N_EXPERTS,), jnp.int32).at[flat_e].add(1)
    padded = (counts + EXPERT_BLOCK - 1) // EXPERT_BLOCK * EXPERT_BLOCK
    pad_end = jnp.cumsum(padded)
    pad_start = pad_end - padded
    start = jnp.cumsum(counts) - counts
    dest = pad_start[se] + jnp.arange(TK, dtype=jnp.int32) - start[se]
    nblk = -(-TK // EXPERT_BLOCK) + N_EXPERTS
    P = nblk * EXPERT_BLOCK
    slot_tok = jnp.full((P,), T, jnp.int32).at[dest].set(flat_tok[order])
    slot_gate = jnp.zeros((P,), F32).at[dest].set(flat_gate[order])
    blk_e = jnp.minimum(jnp.searchsorted(pad_end, jnp.arange(nblk) * EXPERT_BLOCK, side='right'),
                        N_EXPERTS - 1)
    x_pad = jnp.concatenate([xt, jnp.zeros((1, D), xt.dtype)], 0)
    xs = x_pad[slot_tok].reshape(nblk, EXPERT_BLOCK, D)

    def expert_block(args):
        xb, e = args
        gu = xb @ w_gu[e] + b_gu[e]
        gt = jnp.minimum(gu[:, ::2], SWIGLU_LIMIT)
        up = jnp.clip(gu[:, 1::2], -SWIGLU_LIMIT, SWIGLU_LIMIT)
        hid = (up + 1.0) * (gt * jax.nn.sigmoid(gt * SWIGLU_ALPHA))
        return hid @ w_down[e] + b_down[e]

    ys = lax.map(expert_block, (xs, blk_e)).reshape(P, D)
    out = jnp.zeros((T + 1, D), F32).at[slot_tok].add(ys.astype(F32) * slot_gate[:, None])[:T]
    return out.reshape(B, S, D).astype(x.dtype)


def setup_inputs(seed: int = 0) -> dict:
    key = jax.random.key(seed)
    ks = iter(jax.random.split(key, 48))

    def nrm(shape, scale=1.0):
        return jax.random.normal(next(ks), shape, F32) * scale

    def gain(shape):
        return 1.0 + nrm(shape, 0.02)

    d_ab_in = sum(AB_SPLITS)
    d_cd_in = sum(CD_SPLITS)
    n_pool = len(POOL_WINDOWS)
    x = nrm((BATCH, SEQ, D_MODEL))
    p = nrm((DEPTH, BATCH, SEQ, PLE_DIM))
    ab_w_in = nrm((N_EVEN, D_MODEL, d_ab_in), D_MODEL ** -0.5)
    ab_q_norm_g = gain((N_EVEN, R_Q))
    ab_kv_norm_g = gain((N_EVEN, R_KV))
    ab_w_uq = nrm((N_EVEN, R_Q, H_A, D_NOPE), R_Q ** -0.5)
    ab_w_uk = nrm((N_EVEN, R_KV, H_A, D_NOPE), R_KV ** -0.5)
    ab_w_uv = nrm((N_EVEN, R_KV, H_A, D_VA), R_KV ** -0.5)
    ab_w_qidx = nrm((N_EVEN, R_Q, H_IDX, D_IDX), R_Q ** -0.5)
    ab_kidx_norm_g = gain((N_EVEN, D_IDX))
    ab_kidx_norm_b = nrm((N_EVEN, D_IDX), 0.02)
    ab_conv_w = nrm((N_EVEN, CONV_K, 3 * H_B * D_B), CONV_K ** -0.5)
    ab_a_log = jnp.log(jax.random.uniform(next(ks), (N_EVEN, H_B), F32, 1.0, 16.0))
    dt = jnp.exp(jax.random.uniform(next(ks), (N_EVEN, H_B), F32, math.log(1e-3), math.log(1e-1)))
    ab_dt_bias = dt + jnp.log(-jnp.expm1(-dt))
    ab_out_norm_g = gain((N_EVEN, D_B))
    ab_w_out = nrm((N_EVEN, W_AB, D_MODEL), W_AB ** -0.5 * BETA)
    cd_w_in = nrm((N_ODD, D_MODEL, d_cd_in), D_MODEL ** -0.5)
    cd_pool_w = nrm((N_ODD, n_pool, POOL_GROUP, POOL_GROUP), POOL_GROUP ** -0.5)
    cd_pool_scale = gain((N_ODD, n_pool * POOL_GROUP))
    cd_w_out = nrm((N_ODD, W_CD, D_MODEL), W_CD ** -0.5 * BETA)
    ln_mix_g = gain((DEPTH, D_MODEL))
    ln_mix_b = nrm((DEPTH, D_MODEL), 0.02)
    router_w = nrm((DEPTH, D_MODEL, N_EXPERTS), D_MODEL ** -0.5)
    router_b = nrm((DEPTH, N_EXPERTS), 0.01)
    w_gu = nrm((DEPTH, N_EXPERTS, D_MODEL, 2 * D_EXPERT), D_MODEL ** -0.5)
    b_gu = nrm((DEPTH, N_EXPERTS, 2 * D_EXPERT), 0.01)
    w_down = nrm((DEPTH, N_EXPERTS, D_EXPERT, D_MODEL), D_EXPERT ** -0.5 * BETA)
    b_down = nrm((DEPTH, N_EXPERTS, D_MODEL), 0.01)
    ple_w_proj = nrm((DEPTH, PLE_DIM, D_MODEL), PLE_DIM ** -0.5)
    ple_w_gate = nrm((DEPTH, D_MODEL, D_MODEL), D_MODEL ** -0.5)
    ln_ffn_g = gain((DEPTH, D_MODEL))
    ln_ffn_b = nrm((DEPTH, D_MODEL), 0.02)
    return {
        'x': x, 'p': p,
        'ab_w_in': ab_w_in, 'ab_q_norm_g': ab_q_norm_g, 'ab_kv_norm_g': ab_kv_norm_g,
        'ab_w_uq': ab_w_uq, 'ab_w_uk': ab_w_uk, 'ab_w_uv': ab_w_uv, 'ab_w_qidx': ab_w_qidx,
        'ab_kidx_norm_g': ab_kidx_norm_g, 'ab_kidx_norm_b': ab_kidx_norm_b, 'ab_conv_w': ab_conv_w,
        'ab_a_log': ab_a_log, 'ab_dt_bias': ab_dt_bias, 'ab_out_norm_g': ab_out_norm_g, 'ab_w_out': ab_w_out,
        'cd_w_in': cd_w_in, 'cd_pool_w': cd_pool_w, 'cd_pool_scale': cd_pool_scale, 'cd_w_out': cd_w_out,
        'ln_mix_g': ln_mix_g, 'ln_mix_b': ln_mix_b, 'router_w': router_w, 'router_b': router_b,
        'w_gu': w_gu, 'b_gu': b_gu, 'w_down': w_down, 'b_down': b_down,
        'ple_w_proj': ple_w_proj, 'ple_w_gate': ple_w_gate, 'ln_ffn_g': ln_ffn_g, 'ln_ffn_b': ln_ffn_b,
    }


def reference(x, p, ab_w_in, ab_q_norm_g, ab_kv_norm_g, ab_w_uq, ab_w_uk, ab_w_uv, ab_w_qidx,
              ab_kidx_norm_g, ab_kidx_norm_b, ab_conv_w, ab_a_log, ab_dt_bias, ab_out_norm_g, ab_w_out,
              cd_w_in, cd_pool_w, cd_pool_scale, cd_w_out, ln_mix_g, ln_mix_b, router_w, router_b,
              w_gu, b_gu, w_down, b_down, ple_w_proj, ple_w_gate, ln_ffn_g, ln_ffn_b):
    h = x
    for i in range(DEPTH):
        j = i // 2
        if i % 2 == 0:
            mix = _mixer_ab(h, ab_w_in[j], ab_q_norm_g[j], ab_kv_norm_g[j], ab_w_uq[j], ab_w_uk[j],
                            ab_w_uv[j], ab_w_qidx[j], ab_kidx_norm_g[j], ab_kidx_norm_b[j],
                            ab_conv_w[j], ab_a_log[j], ab_dt_bias[j], ab_out_norm_g[j], ab_w_out[j])
        else:
            mix = _mixer_cd(h, cd_w_in[j], cd_pool_w[j], cd_pool_scale[j], cd_w_out[j])
        h = _layernorm(ALPHA * h + mix, ln_mix_g[i], ln_mix_b[i])
        ffn = _moe(h, router_w[i], router_b[i], w_gu[i], b_gu[i], w_down[i], b_down[i])
        ple = jax.nn.sigmoid(h @ ple_w_gate[i]) * (p[i] @ ple_w_proj[i])
        h = _layernorm(ALPHA * h + ffn + ple, ln_ffn_g[i], ln_ffn_b[i])
    return h
```

```python
import numpy as np
import concourse.bass as bass
import concourse.mybir as mybir
from concourse.bass_utils import run_bass_kernel_spmd
from contextlib import ExitStack

F32 = mybir.dt.float32
BF16 = mybir.dt.bfloat16
AF = mybir.ActivationFunctionType
ALU = mybir.AluOpType
AX = mybir.AxisListType

SELF_SYNC = True
NDMA_SEMS = 8


class Tile:
    def __init__(self, ap, name="", bank=None):
        self.ap = ap
        self.name = name
        self.last_write = None
        self.reads = []
        self.bank = bank

    def sub(self, ap, name=""):
        return Tile(ap, name, bank=self.bank)

    def __getitem__(self, idx):
        return View(self, self.ap[idx])

    def v(self, ap):
        return View(self, ap)


class View:
    def __init__(self, tile, ap):
        self.tiles = tile if isinstance(tile, (list, tuple)) else [tile]
        self.ap = ap


def _ap(x):
    return x.ap if isinstance(x, View) else x


class Op:
    __slots__ = ("eng", "fn", "deps", "signal", "val", "sem", "is_dma", "idx", "clock", "waits", "dsem", "cc", "epoch")

    def __init__(self, eng, fn, is_dma=False):
        self.eng = eng
        self.fn = fn
        self.deps = []
        self.signal = False
        self.val = None
        self.sem = None
        self.is_dma = is_dma
        self.waits = []
        self.dsem = None
        self.cc = False


class KB:
    ENGS = ("tensor", "vector", "scalar", "gpsimd", "sync")

    def __init__(self, nc):
        self.nc = nc
        self.ops = {e: [] for e in self.ENGS}
        self.all_ops = []
        self.stack = ExitStack()
        self.gstack = self.stack
        self.dma_rr = {e: 0 for e in self.ENGS}
        self.pending_barrier = {}
        self.since_barrier_dmas = []
        self.pfx = ""
        self.epoch = 0

    def begin_stage(self, pfx):
        self.pfx = pfx
        self.epoch += 1
        self.stack = ExitStack()

    def end_stage(self):
        self.barrier()
        self.stack.close()
        self.stack = self.gstack
        self.pfx = ""

    def barrier(self):
        deps = list(self.since_barrier_dmas)
        for e in self.ENGS:
            if self.ops[e]:
                deps.append(self.ops[e][-1])
        self.since_barrier_dmas = []
        for e in self.ENGS:
            self.pending_barrier[e] = list(deps)

    def collective(self, kind, out_ap, in_ap, groups):
        op = self._rec("gpsimd", lambda e: e.collective_compute(kind, ALU.bypass, replica_groups=groups,
                                                                ins=[in_ap], outs=[out_ap]), [], [], is_dma=True)
        op.cc = True
        return op

    def sbuf(self, name, shape, dt):
        t = self.stack.enter_context(self.nc.sbuf_tensor("sb_" + self.pfx + name, list(shape), dt))
        return Tile(t.ap() if hasattr(t, "ap") and callable(t.ap) else t[:], name)

    def psum(self, name, shape, dt):
        t = self.stack.enter_context(self.nc.psum_tensor("ps_" + self.pfx + name, list(shape), dt))
        return Tile(t.ap() if hasattr(t, "ap") and callable(t.ap) else t[:], name, bank={})

    def _rec(self, eng, fn, reads, writes, is_dma=False):
        op = Op(eng, fn, is_dma)
        op.epoch = self.epoch
        op.idx = len(self.all_ops)
        self.all_ops.append(op)
        self.ops[eng].append(op)
        deps = []
        rt = []
        for r in reads:
            for r1 in (r.tiles if isinstance(r, View) else [r]):
                if isinstance(r1, Tile):
                    rt.append(r1)
                    if r1.last_write is not None:
                        deps.append(r1.last_write)
        wt = []
        for w in writes:
            for w1 in (w.tiles if isinstance(w, View) else [w]):
                if isinstance(w1, Tile):
                    wt.append(w1)
                    if w1.last_write is not None:
                        deps.append(w1.last_write)
                    deps.extend(w1.reads)
        for r in rt:
            r.reads.append(op)
        for w in wt:
            w.last_write = op
            w.reads = []
        for t in rt + wt:
            if t.bank is not None:
                for e2, o2 in t.bank.items():
                    if e2 != eng:
                        deps.append(o2)
                t.bank[eng] = op
        pb = self.pending_barrier.pop(eng, None)
        if pb:
            deps.extend(pb)
        if is_dma:
            self.since_barrier_dmas.append(op)
        op.deps = [d for d in set(deps) if d is not op]
        return op

    def op(self, eng, fn, reads=(), writes=()):
        return self._rec(eng, fn, reads, writes)

    def dma(self, eng, out, in_, **kw):
        o, i = _ap(out), _ap(in_)
        return self._rec(eng, lambda e: e.dma_start(out=o, in_=i, **kw), [in_], [out], is_dma=True)

    def matmul(self, out, lhsT, rhs, start=True, stop=True, **kw):
        o, l, r = _ap(out), _ap(lhsT), _ap(rhs)
        return self._rec("tensor", lambda e: e.matmul(o, l, r, start=start, stop=stop, **kw), [lhsT, rhs], [out])

    def transpose(self, out, in_, ident):
        o, i, d = _ap(out), _ap(in_), _ap(ident)
        return self._rec("tensor", lambda e: e.transpose(o, i, d), [in_, ident], [out])

    def act(self, out, in_, func, bias=None, scale=None, accum_out=None, eng="scalar", extra_reads=()):
        o, i = _ap(out), _ap(in_)
        kw = {}
        reads = [in_] + list(extra_reads)
        writes = [out]
        if bias is not None:
            kw["bias"] = _ap(bias)
            reads.append(bias)
        if scale is not None:
            kw["scale"] = _ap(scale)
            reads.append(scale)
        if accum_out is not None:
            kw["accum_out"] = _ap(accum_out)
            writes.append(accum_out)
        return self._rec(eng, lambda e: e.activation(out=o, in_=i, func=func, **kw), reads, writes)

    def ts(self, eng, out, in0, s1, s2, op0, op1=None, accum_out=None):
        o, i = _ap(out), _ap(in0)
        a1, a2 = _ap(s1), _ap(s2)
        kw = {}
        writes = [out]
        if op1 is not None:
            kw["op1"] = op1
        if accum_out is not None:
            kw["accum_out"] = _ap(accum_out)
            writes.append(accum_out)
        return self._rec(eng, lambda e: e.tensor_scalar(o, i, a1, a2, op0, **kw), [in0, s1, s2], writes)

    def tt(self, eng, out, in0, in1, op):
        o, a, b = _ap(out), _ap(in0), _ap(in1)
        return self._rec(eng, lambda e: e.tensor_tensor(o, a, b, op), [in0, in1], [out])

    def stt(self, eng, out, in0, scalar, in1, op0, op1):
        o, a, s, b = _ap(out), _ap(in0), _ap(scalar), _ap(in1)
        return self._rec(eng, lambda e: e.scalar_tensor_tensor(o, a, s, b, op0, op1), [in0, scalar, in1], [out])

    def copy(self, eng, out, in_):
        o, i = _ap(out), _ap(in_)
        if eng == "scalar":
            return self._rec(eng, lambda e: e.copy(o, i), [in_], [out])
        return self._rec(eng, lambda e: e.tensor_copy(o, i), [in_], [out])

    def memset(self, eng, out, val):
        o = _ap(out)
        return self._rec(eng, lambda e: e.memset(o, val), [], [out])

    def reduce(self, eng, out, in_, op, axis=AX.X):
        o, i = _ap(out), _ap(in_)
        return self._rec(eng, lambda e: e.tensor_reduce(o, i, axis, op), [in_], [out])

    def finish(self, out_ops):
        nc = self.nc
        for op in self.all_ops:
            for d in op.deps:
                if d.eng == op.eng and not d.is_dma:
                    if d.eng == "tensor" or not SELF_SYNC:
                        continue
                d.signal = True
            if op.is_dma:
                op.signal = True
        sems = {(e, ep): self.stack.enter_context(nc.semaphore(f"s_{e}_{ep}")) for e in self.ENGS
                for ep in range(self.epoch + 1)}
        dsems = {e: [self.stack.enter_context(nc.semaphore(f"d_{e}_{i}")) for i in range(NDMA_SEMS)]
                 for e in ("sync", "scalar", "gpsimd")}
        cnt = {key: 0 for key in sems}
        dcnt = {e: [0] * NDMA_SEMS for e in dsems}
        dlast = {e: [None] * NDMA_SEMS for e in dsems}
        drr = {e: 0 for e in dsems}
        ccsem = self.stack.enter_context(nc.semaphore("s_cc"))
        cccnt = 0
        for e in self.ENGS:
            for op in self.ops[e]:
                if op.cc:
                    cccnt += 1
                    op.sem = ccsem
                    op.val = cccnt
                    continue
                if op.is_dma:
                    k = drr[e]
                    drr[e] = (k + 1) % NDMA_SEMS
                    prev = dlast[e][k]
                    if prev is not None:
                        op.deps.append(prev)
                    dcnt[e][k] += 16
                    op.sem = dsems[e][k]
                    op.val = dcnt[e][k]
                    op.dsem = (e, k)
                    dlast[e][k] = op
                elif op.signal:
                    cnt[(e, op.epoch)] += 1
                    op.sem = sems[(e, op.epoch)]
                    op.val = cnt[(e, op.epoch)]
        eng_clock = {e: {} for e in self.ENGS}
        clocks = {}
        for op in self.all_ops:
            e = op.eng
            ck = eng_clock[e]
            for d in sorted(op.deps, key=lambda o: -o.idx):
                if d.sem is None:
                    continue
                if d.eng == e and not d.is_dma and (e == "tensor" or not SELF_SYNC):
                    continue
                key = id(d.sem)
                if ck.get(key, 0) >= d.val:
                    continue
                op.waits.append((d.sem, d.val))
                dc = clocks.get(d.idx)
                if dc:
                    for k2, v2 in dc.items():
                        if ck.get(k2, 0) < v2:
                            ck[k2] = v2
                ck[key] = d.val
            if op.sem is not None:
                c2 = dict(ck)
                c2[id(op.sem)] = max(c2.get(id(op.sem), 0), op.val)
                clocks[op.idx] = c2
        final = {}
        for d in out_ops:
            key = id(d.sem)
            if key not in final or final[key][1] < d.val:
                final[key] = (d.sem, d.val)
        ops = self.ops

        def emit(engine, e):
            for op in ops[e]:
                for (s, v) in op.waits:
                    engine.wait_ge(s, v)
                ins = op.fn(engine)
                if op.cc:
                    ins.then_inc(op.sem, 1)
                elif op.is_dma:
                    ins.then_inc(op.sem, 16)
                elif op.signal:
                    ins.then_inc(op.sem, 1)
            if e == "sync":
                for (s, v) in final.values():
                    engine.wait_ge(s, v)

        with nc.Block() as block:
            @block.tensor
            def _(eng):
                emit(eng, "tensor")

            @block.vector
            def _(eng):
                emit(eng, "vector")

            @block.scalar
            def _(eng):
                emit(eng, "scalar")

            @block.gpsimd
            def _(eng):
                emit(eng, "gpsimd")

            @block.sync
            def _(eng):
                emit(eng, "sync")
        self.stack.close()

import os
DBG = os.environ.get('MOE_DBG', '')

ALPHA_ = (2 * 2) ** 0.25
LIM = 7.0
SA = 1.702
import math
SGMAX = 1.0 / (1.0 + math.exp(-SA * LIM))


def ln_rows(k, src, dst_tile_ap, scr, st, lng_s, lnb_s, eps):
    D = 1024
    k.reduce("vector", st["s1"][:], src, ALU.add)
    k.act(scr[:], src, AF.Square, accum_out=st["s2"][:])
    k.ts("vector", st["mean"][:], st["s1"][:], 1.0 / D, None, ALU.mult)
    k.tt("vector", st["msq"][:], st["mean"][:], st["mean"][:], ALU.mult)
    k.stt("vector", st["var"][:], st["s2"][:], 1.0 / D, st["msq"][:], ALU.mult, ALU.subtract)
    k.ts("vector", st["var"][:], st["var"][:], eps, None, ALU.add)
    k.act(st["sd"][:], st["var"][:], AF.Sqrt)
    k.op("vector", (lambda o, i: (lambda e: e.reciprocal(o, i)))(st["rstd"].ap, st["sd"].ap), [st["sd"]], [st["rstd"]])
    k.stt("vector", st["nmr"][:], st["mean"][:], -1.0, st["rstd"][:], ALU.mult, ALU.mult)
    k.act(scr[:], src, AF.Identity, bias=st["nmr"][:], scale=st["rstd"][:])
    k.tt("vector", scr[:], scr[:], lng_s[:], ALU.mult)
    k.tt("vector", dst_tile_ap, scr[:], lnb_s[:], ALU.add)


def build_moe(NTOK=2048, TP=1024, NE=32, k=None, dm=None, pfx=""):
    standalone = k is None
    if standalone:
        k = KB(bass.Bass("TRN2", target_bir_lowering=False))
    nc = k.nc
    dm = dm or {}
    D = 1024
    NT = TP // 128
    NG = TP // 512
    NPASS = NTOK // TP

    def din(name, shape):
        if name in dm:
            return dm[name]
        return nc.dram_tensor(pfx + name, list(shape), F32, kind="ExternalInput").ap()
    h_d = din("h", [NTOK, D]); pT_d = din("pT", [256, NTOK])
    rw_d = din("rw", [D, 32]); rb_d = din("rb", [1, 32])
    wg_d = din("wg", [32, D, 1024]); wu_d = din("wu", [32, D, 1024])
    bg_d = din("bg", [128, 32, 8]); bu_d = din("bu", [128, 32, 8])
    wd_d = din("wd", [32, 1024, D]); bd_d = din("bd", [32, D])
    pwp_d = din("pwp", [256, D]); pwg_d = din("pwg", [D, D])
    lng_d = din("lng", [1, D]); lnb_d = din("lnb", [1, D]); id_d = din("ident", [128, 128])
    out_d = dm["out"] if "out" in dm else nc.dram_tensor("out", [NTOK, D], F32, kind="ExternalOutput").ap()

    if not standalone:
        k.begin_stage(pfx)
    ident = k.sbuf("ident", [128, 128], F32)
    ones1 = k.sbuf("ones1", [1, 128], F32)
    rw_s = k.sbuf("rw_s", [128, 8, 32], F32)
    rb_s = k.sbuf("rb_s", [1, 32], F32)
    bd_s = k.sbuf("bd_s", [32, D], F32)
    bg_s = k.sbuf("bg_s", [128, 32, 8], F32)
    bgs_s = k.sbuf("bgs_s", [128, 32, 8], F32)
    bu_s = k.sbuf("bu_s", [128, 32, 8], F32)
    pwg_s = k.sbuf("pwg_s", [128, 8, D], BF16)
    pwp_s = k.sbuf("pwp_s", [128, 2, D], BF16)
    lng_s = k.sbuf("lng_s", [128, D], F32)
    lnb_s = k.sbuf("lnb_s", [128, D], F32)
    pT_s = k.sbuf("pT_s", [128, 2, TP], BF16)
    hs = [k.sbuf(f"hs{i}", [128, D], F32) for i in range(2)]
    hT32 = k.sbuf("hT32", [128, 8, 128], F32)
    hT_all = k.sbuf("hT", [128, 8, TP], BF16)
    hT = [Tile(hT_all.ap[:, :, g * 512:(g + 1) * 512], f"hT{g}") for g in range(NG)]
    hid_all = k.sbuf("hidT", [128, 8, TP], BF16)
    hidT = [[Tile(hid_all.ap[:, j, g * 512:(g + 1) * 512], f"hid{j}_{g}") for g in range(NG)] for j in range(8)]
    acc_all = k.sbuf("acc", [128, NT, D], F32)
    acc = [Tile(acc_all.ap[:, t, :], f"acc{t}") for t in range(NT)]
    gate_all = k.sbuf("gate", [128, NT, 32], F32)
    gate = [Tile(gate_all.ap[:, t, :], f"gate{t}") for t in range(NT)]
    NPIECE = 4
    wgp = [k.sbuf(f"wgp{i}", [128, 8, 128], BF16) for i in range(NPIECE)]
    wup = [k.sbuf(f"wup{i}", [128, 8, 128], BF16) for i in range(NPIECE)]
    wdb = [k.sbuf(f"wdb{i}", [128, 8, D], BF16) for i in range(2)]
    tmp = {n: [k.sbuf(f"tmp{n}{i}", [128, 512], F32) for i in range(2)] for n in "ABCD"}
    scr = k.sbuf("scr", [128, D], F32)
    small = {n: k.sbuf("sm_" + n, [128, 1], F32) for n in
             ("s1", "s2", "mean", "msq", "var", "sd", "rstd", "nmr", "negm", "den", "rden")}
    lg = k.sbuf("lg", [128, 32], F32)
    ex = k.sbuf("ex", [128, 32], F32)
    em = k.sbuf("em", [128, 32], F32)
    top8 = k.sbuf("top8", [128, 8], F32)
    gT_s = k.sbuf("gT_s", [32, 128], F32)
    PB = [k.psum(f"pb{i}", [128, 512], F32) for i in range(8)]
    Gb, Ub, Yb = PB[0:2], PB[2:4], PB[4:6]
    tr = PB[0:2]; pg, pp, pbias, misc = PB[2], PB[3], PB[4], PB[6]
    lgp = misc.sub(misc.ap[:, 0:32], "lgp")
    gTp = misc.sub(misc.ap[0:32, 128:256], "gTp")

    k.dma("sync", ident[:], id_d)
    k.memset("vector", ones1[:], 1.0)
    k.dma("sync", rw_s[:], rw_d.rearrange("(c p) n -> p c n", p=128))
    k.dma("sync", rb_s[:], rb_d)
    k.dma("sync", bd_s[:], bd_d)
    k.dma("sync", bg_s[:], bg_d)
    k.dma("sync", bu_s[:], bu_d)
    k.ts("vector", bu_s[:], bu_s[:], 1.0, None, ALU.add)
    k.ts("vector", bgs_s[:], bg_s[:], SA, None, ALU.mult)
    k.dma("gpsimd", pwg_s[:], pwg_d.rearrange("(c p) n -> p c n", p=128))
    k.dma("gpsimd", pwp_s[:], pwp_d.rearrange("(c p) n -> p c n", p=128))
    k.dma("sync", lng_s[:], lng_d.partition_broadcast(128))
    k.dma("sync", lnb_s[:], lnb_d.partition_broadcast(128))

    out_ops = []
    q = 0
    pi = 0
    ei = 0
    for ps in range(NPASS):
        tok0 = ps * TP
        k.dma("gpsimd", pT_s[:], pT_d[:, tok0:tok0 + TP].rearrange("(c p) t -> p c t", p=128))
        for t in range(NT):
            g = t // 4
            tl = slice((t % 4) * 128, (t % 4 + 1) * 128)
            hsb = hs[t % 2]
            k.dma("sync", hsb[:], h_d[tok0 + t * 128: tok0 + (t + 1) * 128, :])
            for c in range(8):
                k.transpose(tr[c // 4][:, (c % 4) * 128:(c % 4 + 1) * 128], hsb[:, c * 128:(c + 1) * 128], ident[:])
            for hh in range(2):
                src = tr[hh].ap.rearrange("p (c m) -> p c m", c=4)
                k.copy("vector", hT32[:, hh * 4:(hh + 1) * 4, :], tr[hh].v(src))
                k.copy("scalar", hT[g][:, hh * 4:(hh + 1) * 4, tl], hT32[:, hh * 4:(hh + 1) * 4, :])
            for c in range(8):
                k.matmul(lgp[:], hT32[:, c, :], rw_s[:, c, :], start=(c == 0), stop=False)
            k.matmul(lgp[:], ones1[0:1, :], rb_s[0:1, :], start=False, stop=True)
            k.copy("vector", lg[:], lgp[:])
            k.op("vector", (lambda o, i: (lambda e: e.max(o, i)))(top8.ap, lg.ap), [lg], [top8])
            k.ts("vector", small["negm"][:], top8[:, 0:1], -1.0, None, ALU.mult)
            k.act(ex[:], lg[:], AF.Exp, bias=small["negm"][:])
            k.stt("vector", em[:], lg[:], top8[:, 3:4], ex[:], ALU.is_ge, ALU.mult)
            k.reduce("vector", small["den"][:], em[:], ALU.add)
            k.op("vector", (lambda o, i: (lambda e: e.reciprocal(o, i)))(small["rden"].ap, small["den"].ap),
                 [small["den"]], [small["rden"]])
            k.ts("vector", gate[t][:], em[:], small["rden"][:], None, ALU.mult)
            k.transpose(gTp[:], gate[t][:], ident[:])
            k.copy("vector", gT_s[:], gTp[:])
            for half in range(2):
                hsl = slice(half * 512, (half + 1) * 512)
                for c in range(8):
                    k.matmul(pg[:], hT[g][:, c, tl], pwg_s[:, c, hsl], start=(c == 0), stop=(c == 7))
                for c in range(2):
                    k.matmul(pp[:], pT_s[:, c, t * 128:(t + 1) * 128], pwp_s[:, c, hsl], start=(c == 0), stop=(c == 1))
                k.matmul(pbias[:], gT_s[:], bd_s[:, hsl], start=True, stop=True)
                tA = tmp["A"][half]
                k.act(tA[:], pg[:], AF.Sigmoid)
                k.tt("vector", tA[:], tA[:], pp[:], ALU.mult)
                k.stt("vector", tA[:], hsb[:, hsl], ALPHA_, tA[:], ALU.mult, ALU.add)
                k.tt("vector", acc[t][:, hsl], tA[:], pbias[:], ALU.add)
        for e in range(NE):
            wd_t = wdb[q % 2]
            q += 1
            for hh in range(2):
                k.dma("gpsimd", wd_t[:, hh * 4:(hh + 1) * 4, :],
                      wd_d[e, hh * 512:(hh + 1) * 512, :].rearrange("(c p) n -> p c n", p=128))
            for j in range(0 if 'nogu' not in DBG else 99, 8):
                wg_t = wgp[pi % NPIECE]; wu_t = wup[pi % NPIECE]
                pi += 1
                k.dma("gpsimd", wg_t[:], wg_d[e].rearrange("(c p) m -> p c m", p=128)[:, :, j * 128:(j + 1) * 128])
                k.dma("gpsimd", wu_t[:], wu_d[e].rearrange("(c p) m -> p c m", p=128)[:, :, j * 128:(j + 1) * 128])
                for g in range(NG):
                    Gp = Gb[ei % 2]; Up = Ub[ei % 2]
                    tA, tB, tC = tmp["A"][ei % 2], tmp["B"][ei % 2], tmp["C"][ei % 2]
                    ei += 1
                    for c in range(8):
                        k.matmul(Gp[:], wg_t[:, c, :], hT[g][:, c, :], start=(c == 0), stop=(c == 7))
                    for c in range(8):
                        k.matmul(Up[:], wu_t[:, c, :], hT[g][:, c, :], start=(c == 0), stop=(c == 7))
                    k.ts("vector", tA[:], Gp[:], bg_s[:, e, j:j + 1], LIM, ALU.add, ALU.min)
                    k.act(tB[:], tA[:], AF.Sigmoid, scale=SA)
                    k.ts("vector", tC[:], Up[:], bu_s[:, e, j:j + 1], LIM + 1.0, ALU.add, ALU.min)
                    k.stt("vector", tC[:], tC[:], -LIM + 1.0, tA[:], ALU.max, ALU.mult)
                    k.tt("vector", hidT[j][g][:], tB[:], tC[:], ALU.mult)
            for t in range(NT if 'nodown' not in DBG else 0):
                g = t // 4
                tl = slice((t % 4) * 128, (t % 4 + 1) * 128)
                for half in range(2):
                    hsl = slice(half * 512, (half + 1) * 512)
                    Yp = Yb[(t * 2 + half) % 2]
                    for j in range(8):
                        k.matmul(Yp[:], hidT[j][g][:, tl], wd_t[:, j, hsl], start=(j == 0), stop=(j == 7))
                    k.stt("vector", acc[t][:, hsl], Yp[:], gate[t][:, e:e + 1], acc[t][:, hsl], ALU.mult, ALU.add)
        for t in range(NT):
            ob = hs[t % 2]
            ln_rows(k, acc[t][:], ob[:], scr, small, lng_s, lnb_s, 1e-5)
            out_ops.append(k.dma("sync", out_d[tok0 + t * 128: tok0 + (t + 1) * 128, :], ob[:]))
    if standalone:
        k.finish(out_ops)
        return nc
    k.end_stage()
    return out_ops

import os
CDBG = os.environ.get('CD_DBG', '')

NDELTA = 17


def cd_masks():
    k = np.arange(128)[:, None, None]
    dl = np.arange(NDELTA)[None, :, None]
    q = np.arange(128)[None, None, :]
    dist = 128 * dl + q - k
    c = ((dist >= 0) & (dist <= 128)).astype(np.float32)
    c += ((dist >= 0) & (dist <= 512) & (dist % 4 == 0))
    c += ((dist >= 0) & (dist <= 2048) & (dist % 16 == 0))
    return np.ascontiguousarray(np.broadcast_to(c[:, :, None, :], (128, NDELTA, 2, 128))).astype(np.float32)


def build_cd(NTOK=2048, HALO=2048, NPAIR=6, k=None, dm=None, pfx=""):
    standalone = k is None
    if standalone:
        k = KB(bass.Bass("TRN2", target_bir_lowering=False))
    nc = k.nc
    dm = dm or {}
    D = 1024
    NTT = (NTOK + HALO) // 128
    NH = HALO // 128
    NQ = NTOK // 128
    NGA = (NTOK + HALO) // 512
    NGH = HALO // 512

    def din(name, shape):
        if name in dm:
            return dm[name]
        return nc.dram_tensor(pfx + name, list(shape), F32, kind="ExternalInput").ap()
    hx_d = din("hx", [HALO + NTOK, D])
    win_d = din("w_in", [D, 2560]); pw_d = din("pool_w", [4, 64, 64]); psc_d = din("pool_scale", [128, 2])
    wout_d = din("w_out", [D, D]); lng_d = din("lng", [1, D]); lnb_d = din("lnb", [1, D])
    id_d = din("ident", [128, 128]); mask_d = din("masks", [128, NDELTA, 2, 128])
    valid_d = din("valid", [128, 1]); invcnt_d = din("invcnt", [128, 2, 16])
    out_d = dm["out"] if "out" in dm else nc.dram_tensor("out", [NTOK, D], F32, kind="ExternalOutput").ap()

    if not standalone:
        k.begin_stage(pfx)
    ident = k.sbuf("ident", [128, 128], F32)
    masks = k.sbuf("masks", [128, NDELTA, 2, 128], BF16)
    valid = k.sbuf("valid", [128, 1], F32)
    invcnt = k.sbuf("invcnt", [128, 2, 16], F32)
    psc = k.sbuf("psc", [128, 2], F32)
    pwbd32 = k.sbuf("pwbd32", [128, 2, 128], F32)
    pwbd = k.sbuf("pwbd", [128, 2, 128], BF16)
    wud = k.sbuf("wud", [128, 8, 256], BF16)
    wout = k.sbuf("wout", [128, 8, D], BF16)
    lng_s = k.sbuf("lng_s", [128, D], F32)
    lnb_s = k.sbuf("lnb_s", [128, D], F32)
    hT = k.sbuf("hT", [128, 8, HALO + NTOK], BF16)
    hs = [k.sbuf(f"hs{i}", [128, D], F32) for i in range(2)]
    KT = k.sbuf("KT", [128, HALO + NTOK], BF16)
    QT = k.sbuf("QT", [128, NQ, 2, 128], BF16)
    Vx = k.sbuf("Vx", [128, NTT, 2, 65], BF16)
    wq = [k.sbuf(f"wq{i}", [128, 8, 128], BF16) for i in range(2)]
    wk = [k.sbuf(f"wk{i}", [128, 8, 128], BF16) for i in range(2)]
    wv = [k.sbuf(f"wv{i}", [128, 8, 128], BF16) for i in range(2)]
    oT_all = k.sbuf("oT", [128, 8, NTOK], BF16)
    oT = [Tile(oT_all.ap[:, c, :], f"oT{c}") for c in range(8)]
    PG = 256
    xg = k.sbuf("xg", [128, 2, 16 + PG], F32)
    pA = k.sbuf("pA", [128, 2, 16 + PG], F32)
    pB = k.sbuf("pB", [128, 2, 16 + PG], F32)
    dm = k.sbuf("dm", [128, 2, PG], BF16)
    ex = [k.sbuf(f"ex{i}", [128, 2, 128], BF16) for i in range(3)]
    pT = [k.sbuf(f"pT{i}", [128, 2, 128], BF16) for i in range(3)]
    on = [k.sbuf(f"on{i}", [128, 2, 64], F32) for i in range(2)]
    rden = [k.sbuf(f"rden{i}", [128, 2, 1], F32) for i in range(2)]
    scr = k.sbuf("scr", [128, D], F32)
    acc = k.sbuf("acc", [128, D], F32)
    small = {n: k.sbuf("sm_" + n, [128, 1], F32) for n in
             ("s1", "s2", "mean", "msq", "var", "sd", "rstd", "nmr")}
    PB = [k.psum(f"pb{i}", [128, 512], F32) for i in range(8)]

    k.dma("sync", ident[:], id_d)
    k.dma("gpsimd", masks[:], mask_d)
    k.dma("sync", valid[:], valid_d)
    k.dma("sync", invcnt[:], invcnt_d)
    k.dma("sync", psc[:], psc_d)
    k.memset("vector", pwbd32[:], 0.0)
    for g in range(4):
        r = 64 * (g % 2)
        k.dma("sync", pwbd32[r:r + 64, g // 2, r:r + 64], pw_d[g])
    k.copy("vector", pwbd[:], pwbd32[:])
    k.dma("gpsimd", wud[:], win_d.rearrange("(c p) n -> p c n", p=128)[:, :, 2304:2560])
    k.dma("gpsimd", wout[:], wout_d.rearrange("(c p) n -> p c n", p=128))
    k.dma("sync", lng_s[:], lng_d.partition_broadcast(128))
    k.dma("sync", lnb_s[:], lnb_d.partition_broadcast(128))
    k.memset("vector", QT[:], 0.0)
    k.memset("vector", Vx[:, :, :, 64:65], 1.0)
    k.ts("vector", Vx[:, 0:NH, :, 64:65], Vx[:, 0:NH, :, 64:65], valid[:, 0:1], None, ALU.mult)

    for ti in range(NTT):
        hsb = hs[ti % 2]
        k.dma("sync", hsb[:], hx_d[ti * 128:(ti + 1) * 128, :])
        for c in range(8):
            k.transpose(PB[c // 4][:, (c % 4) * 128:(c % 4 + 1) * 128], hsb[:, c * 128:(c + 1) * 128], ident[:])
        for hh in range(2):
            src = PB[hh].v(PB[hh].ap.rearrange("p (c m) -> p c m", c=4))
            k.copy("vector" if hh == 0 else "scalar", hT[:, hh * 4:(hh + 1) * 4, ti * 128:(ti + 1) * 128], src)

    for g in range(0 if 'nopool' in CDBG else NTOK // PG):
        t0 = HALO + g * PG
        PE_ = 16 + PG
        for i in range(2):
            pu = PB[6 + i]
            for c in range(8):
                k.matmul(pu[:, 0:PG], wud[:, c, i * 128:(i + 1) * 128], hT[:, c, t0:t0 + PG], start=(c == 0), stop=(c == 7))
            k.copy("vector", xg[:, i, 16:PE_], pu[:, 0:PG])
            ph = PB[4 + i]
            for c in range(8):
                k.matmul(ph[:, 0:16], wud[:, c, i * 128:(i + 1) * 128], hT[:, c, t0 - 16:t0], start=(c == 0), stop=(c == 7))
            k.copy("vector", xg[:, i, 0:16], ph[:, 0:16])
        E = "gpsimd"
        k.tt(E, pA[:, :, 1:PE_], xg[:, :, 1:PE_], xg[:, :, 0:PE_ - 1], ALU.add)
        k.tt(E, pB[:, :, 3:PE_], pA[:, :, 3:PE_], pA[:, :, 1:PE_ - 2], ALU.add)
        k.tt(E, pA[:, 1, 7:PE_], pB[:, 1, 7:PE_], pB[:, 1, 3:PE_ - 4], ALU.add)
        k.tt(E, pB[64:128, 1, 15:PE_], pA[64:128, 1, 15:PE_], pA[64:128, 1, 7:PE_ - 8], ALU.add)
        srcs = [(pA, 0, slice(0, 64), 0.5), (pB, 0, slice(64, 128), 0.25),
                (pA, 1, slice(0, 64), 0.125), (pB, 1, slice(64, 128), 1.0 / 16)]
        for (st_, i, pr, iw) in srcs:
            if g == 0:
                k.tt("vector", st_[pr, i, 16:32], st_[pr, i, 16:32], invcnt[pr, i, :], ALU.mult)
                k.tt("vector", xg[pr, i, 16:32], st_[pr, i, 16:32], xg[pr, i, 16:32], ALU.subtract)
                k.stt("vector", xg[pr, i, 32:PE_], st_[pr, i, 32:PE_], iw, xg[pr, i, 32:PE_], ALU.mult, ALU.subtract)
            else:
                k.stt("vector", xg[pr, i, 16:PE_], st_[pr, i, 16:PE_], iw, xg[pr, i, 16:PE_], ALU.mult, ALU.subtract)
        k.copy("vector", dm[:], xg[:, :, 16:PE_])
        for i in range(2):
            po = PB[6 + i]
            k.matmul(po[:, 0:PG], pwbd[:, i, :], dm[:, i, :], start=True, stop=True)
            k.ts("vector", oT[6 + i][:, g * PG:(g + 1) * PG], po[:, 0:PG], psc[:, i:i + 1], None, ALU.mult)

    si = 0
    for hp in range(NPAIR):
        wq_t, wk_t, wv_t = wq[hp % 2], wk[hp % 2], wv[hp % 2]
        wv3 = win_d.rearrange("(c p) n -> p c n", p=128)
        k.dma("gpsimd", wq_t[:], wv3[:, :, hp * 128:(hp + 1) * 128])
        k.dma("gpsimd", wk_t[:], wv3[:, :, 768 + hp * 128:768 + (hp + 1) * 128])
        k.dma("gpsimd", wv_t[:], wv3[:, :, 1536 + hp * 128:1536 + (hp + 1) * 128])
        for G in range(NGA):
            pk = PB[G % 2]
            for c in range(8):
                k.matmul(pk[:], wk_t[:, c, :], hT[:, c, G * 512:(G + 1) * 512], start=(c == 0), stop=(c == 7))
            k.copy("scalar" if G % 2 else "vector", KT[:, G * 512:(G + 1) * 512], pk[:])
        for G in range(NGH, NGA):
            pq = PB[G % 2]
            for c in range(8):
                k.matmul(pq[:], wq_t[:, c, :], hT[:, c, G * 512:(G + 1) * 512], start=(c == 0), stop=(c == 7))
            g4 = (G - NGH) * 4
            for h in range(2):
                pr = slice(64 * h, 64 * h + 64)
                k.copy("scalar" if h else "vector", QT[pr, g4:g4 + 4, h, :],
                       pq.v(pq.ap[pr, :].rearrange("p (t q) -> p t q", t=4)))
        for G in range(NGA):
            pv = PB[G % 2]
            for tt_ in range(4):
                ti = G * 4 + tt_
                for c in range(8):
                    k.matmul(pv[:, tt_ * 128:(tt_ + 1) * 128], hT[:, c, ti * 128:(ti + 1) * 128], wv_t[:, c, :],
                             start=(c == 0), stop=(c == 7))
            src = pv.v(pv.ap.rearrange("p (t h d) -> p t h d", t=4, h=2))
            if G < NGH:
                k.ts("vector", Vx[:, G * 4:(G + 1) * 4, :, 0:64], src, valid[:, 0:1], None, ALU.mult)
            else:
                k.copy("vector", Vx[:, G * 4:(G + 1) * 4, :, 0:64], src)
        pend = []

        def flush(n_keep):
            while len(pend) > n_keep:
                pend.pop(0)()
        for qt in range(0 if 'noattn' in CDBG else NQ):
            ob = PB[4 + qt % 2]
            for dl in range(NDELTA):
                kt = qt + NH - dl
                if kt < 0:
                    continue
                sb = PB[2 + si % 2]
                s3 = sb.v(sb.ap[:, 0:256].rearrange("p (h q) -> p h q", h=2))
                k.matmul(sb[:, 0:256], KT[:, kt * 128:(kt + 1) * 128],
                         QT.v(QT.ap[:, qt, :, :].rearrange("p h q -> p (h q)")), start=True, stop=True)
                ext = ex[si % 3]; ptt = pT[si % 3]
                si += 1
                k.act(ext[:], s3, AF.Exp, scale=0.125)
                k.tt("vector", ptt[:], ext[:], masks[:, dl, :, :], ALU.mult)

                def pv(ob=ob, ptt=ptt, kt=kt, dl=dl):
                    for h in range(2):
                        first = (dl == 0 and h == 0)
                        k.matmul(ob[:, h * 65:(h + 1) * 65], ptt[:, h, :], Vx[:, kt, h, :], start=first,
                                 stop=(dl == NDELTA - 1), skip_group_check=True)
                pend.append(pv)
                flush(2)

            def fin(ob=ob, qt=qt, hp=hp):
                o3 = ob.v(ob.ap[:, 0:130].rearrange("p (h d) -> p h d", h=2))
                rd = rden[qt % 2]; onn = on[qt % 2]
                k.op("vector", (lambda o, i: (lambda e: e.reciprocal(o, i)))(rd.ap, o3.ap[:, :, 64:65]), [ob], [rd])
                for h in range(2):
                    k.ts("vector", onn[:, h, :], ob[:, h * 65:h * 65 + 64], rd[:, h, :], None, ALU.mult)
                ptr = PB[6 + qt % 2]
                k.transpose(ptr[:, 0:128], onn.v(onn.ap.rearrange("p h d -> p (h d)")), ident[:])
                k.copy("scalar", oT[hp][:, qt * 128:(qt + 1) * 128], ptr[:, 0:128])
            pend.append(fin)
        flush(0)

    out_ops = []
    for t in range(NQ):
        hsb = hs[t % 2]
        k.dma("sync", hsb[:], hx_d[HALO + t * 128:HALO + (t + 1) * 128, :])
        for half in range(2):
            hsl = slice(half * 512, (half + 1) * 512)
            pm = PB[half]
            for c in range(8):
                k.matmul(pm[:], oT[c][:, t * 128:(t + 1) * 128], wout[:, c, hsl], start=(c == 0), stop=(c == 7))
            k.stt("vector", acc[:, hsl], hsb[:, hsl], ALPHA_, pm[:], ALU.mult, ALU.add)
        ln_rows(k, acc[:], hsb[:], scr, small, lng_s, lnb_s, 1e-5)
        out_ops.append(k.dma("sync", out_d[t * 128:(t + 1) * 128, :], hsb[:]))
    if standalone:
        k.finish(out_ops)
        return nc
    k.end_stage()
    return out_ops


NEG = -1.0e30
TOPK = 256
NIT = 16


def recip(k, eng, out, in_):
    o, i = _ap(out), _ap(in_)
    return k.op(eng, lambda e: e.reciprocal(o, i), [in_], [out])


def dsa_slots(r):
    sl = []
    for m in range(8):
        sl.append(8 * m + r)
        sl.append(8 * m + 7 - r)
    return sl


def dsa_nb(slot):
    m, hi = slot // 2, slot % 2
    return 8 * m + (8 if hi else 4)


def build_dsa(NSLOT=16, S=8192, k=None, dm=None, pfx=""):
    standalone = k is None
    if standalone:
        k = KB(bass.Bass("TRN2", target_bir_lowering=False))
    nc = k.nc
    dm = dm or {}
    NKT = S // 128

    def din(name, shape):
        if name in dm:
            return dm[name]
        return nc.dram_tensor(pfx + name, list(shape), F32, kind="ExternalInput").ap()
    xT_d = din("xT", [1024, S])
    xqT_d = din("xqT", [1024, NSLOT * 128])
    wkv_d = din("wkv", [1024, 192])
    wq_d = din("wq", [1024, 264])
    gq_d = din("gq", [1, 256]); gkv_d = din("gkv", [1, 128]); gki_d = din("gki", [1, 64]); bki_d = din("bki", [1, 64])
    wuqT_d = din("wuqT", [64, 8, 256])
    wukT_d = din("wukT", [64, 8, 128])
    wqidx_d = din("wqidx", [256, 512])
    wuv_d = din("wuv", [128, 512])
    cb_d = din("cb", [128, 2, 512])
    id_d = din("ident", [128, 128])
    out_d = dm["out"] if "out" in dm else nc.dram_tensor("out", [NSLOT * 128, 512], F32, kind="ExternalOutput").ap()

    if not standalone:
        k.begin_stage(pfx)
    ident = k.sbuf("ident", [128, 128], F32)
    identb = k.sbuf("identb", [128, 128], BF16)
    wkv = k.sbuf("wkv", [128, 8, 192], BF16)
    wq = k.sbuf("wq", [128, 8, 264], BF16)
    gq = k.sbuf("gq", [128, 256], F32); gkv = k.sbuf("gkv", [128, 128], F32)
    gki = k.sbuf("gki", [128, 64], F32); bki = k.sbuf("bki", [128, 64], F32)
    wuqT = k.sbuf("wuqT", [64, 8, 256], F32); wukT = k.sbuf("wukT", [64, 8, 128], F32)
    wqidx = k.sbuf("wqidx", [128, 2, 512], F32)
    wqlat = k.sbuf("wqlat", [128, 2, 8, 128], BF16)
    wuv = k.sbuf("wuv", [128, 512], BF16)
    cb = k.sbuf("cb", [128, 2, 512], F32)
    ckvx = k.sbuf("ckvx", [128, NKT, 129], BF16)
    ckvT = k.sbuf("ckvT", [128, S], BF16)
    kidxT = k.sbuf("kidxT", [64, S], F32)
    isc = k.sbuf("isc", [128, S], F32)
    mask = k.sbuf("mask", [128, S], BF16)
    maskT = k.sbuf("maskT", [128, NKT, 128], BF16)
    xtb = [k.sbuf(f"xtb{i}", [128, 8, 128], BF16) for i in range(2)]
    kv = [k.sbuf(f"kv{i}", [128, 192], F32) for i in range(2)]
    c32 = [k.sbuf(f"c32{i}", [128, 128], F32) for i in range(2)]
    ki32 = [k.sbuf(f"ki32{i}", [128, 64], F32) for i in range(2)]
    scr = k.sbuf("scr", [128, 256], F32)
    cq = k.sbuf("cq", [128, 264], F32)
    cqn = k.sbuf("cqn", [128, 256], F32)
    cqT32 = k.sbuf("cqT32", [128, 2, 128], F32)
    cqTb = k.sbuf("cqTb", [128, 2, 128], BF16)
    qidxT = k.sbuf("qidxT", [64, 8, 128], F32)
    qlatT = k.sbuf("qlatT", [128, 8, 128], BF16)
    rl = [k.sbuf(f"rl{i}", [128, 512], F32) for i in range(3)]
    ex = [k.sbuf(f"ex{i}", [128, 4, 128], BF16) for i in range(3)]
    pT = [k.sbuf(f"pT{i}", [128, 4, 128], BF16) for i in range(3)]
    mask_alias = Tile(mask.ap, "mask_alias")
    oln = k.sbuf("oln", [128, 8, 128], F32)
    olT = k.sbuf("olT", [128, 8, 128], BF16)
    oa = [k.sbuf(f"oa{i}", [128, 512], F32) for i in range(2)]
    sm = {n: k.sbuf("sm_" + n, [128, 1], F32) for n in
          ("s1", "s2", "mean", "msq", "var", "sd", "rstd", "nmr", "lo", "hi", "mid", "cnt", "ge", "d1", "d2", "mn", "mx")}
    rd8 = k.sbuf("rd8", [128, 8, 1], F32)
    PB = [k.psum(f"pb{i}", [128, 512], F32) for i in range(7)]
    PBb = k.psum("pbb", [128, 1024], BF16)

    k.dma("sync", ident[:], id_d)
    k.copy("vector", identb[:], ident[:])
    k.dma("gpsimd", wkv[:], wkv_d.rearrange("(c p) n -> p c n", p=128))
    k.dma("gpsimd", wq[:], wq_d.rearrange("(c p) n -> p c n", p=128))
    k.dma("sync", gq[:], gq_d.partition_broadcast(128)); k.dma("sync", gkv[:], gkv_d.partition_broadcast(128))
    k.dma("sync", gki[:], gki_d.partition_broadcast(128)); k.dma("sync", bki[:], bki_d.partition_broadcast(128))
    k.dma("sync", wuqT[:], wuqT_d); k.dma("sync", wukT[:], wukT_d)
    k.dma("sync", wqidx[:], wqidx_d.rearrange("(c p) n -> p c n", p=128))
    k.dma("gpsimd", wuv[:], wuv_d)
    k.dma("sync", cb[:], cb_d)
    k.memset("vector", ckvx[:, :, 128:129], 1.0)
    for h in range(8):
        for rc in range(2):
            pw = PB[(h * 2 + rc) % 2]
            k.matmul(pw[:, 0:128], wuqT[:, h, rc * 128:(rc + 1) * 128], wukT[:, h, :], start=True, stop=True)
            k.copy("vector", wqlat[:, rc, h, :], pw[:, 0:128])

    def rms_stats(x_view, D, eps):
        k.act(scr[:, 0:D], x_view, AF.Square, accum_out=sm["s2"][:])
        k.ts("vector", sm["var"][:], sm["s2"][:], 1.0 / D, eps, ALU.mult, ALU.add)
        k.act(sm["sd"][:], sm["var"][:], AF.Sqrt)
        recip(k, "vector", sm["rstd"][:], sm["sd"][:])

    for ti in range(NKT):
        xt = xtb[ti % 2]; kvb = kv[ti % 2]; cc = c32[ti % 2]; ki = ki32[ti % 2]
        k.dma("gpsimd", xt[:], xT_d.rearrange("(c p) t -> p c t", p=128)[:, :, ti * 128:(ti + 1) * 128])
        pk = PB[ti % 2]
        for c in range(8):
            k.matmul(pk[:, 0:192], xt[:, c, :], wkv[:, c, :], start=(c == 0), stop=(c == 7))
        k.copy("scalar", kvb[:], pk[:, 0:192])
        rms_stats(kvb[:, 0:128], 128, 1e-6)
        k.stt("vector", cc[:], kvb[:, 0:128], sm["rstd"][:], gkv[:], ALU.mult, ALU.mult)
        k.copy("scalar", ckvx[:, ti, 0:128], cc[:])
        pt_ = PB[2 + ti % 2]
        k.transpose(pt_[:, 0:128], cc[:], ident[:])
        k.copy("vector", ckvT[:, ti * 128:(ti + 1) * 128], pt_[:, 0:128])
        k.reduce("vector", sm["s1"][:], kvb[:, 128:192], ALU.add)
        k.act(scr[:, 0:64], kvb[:, 128:192], AF.Square, accum_out=sm["s2"][:])
        k.ts("vector", sm["mean"][:], sm["s1"][:], 1.0 / 64, None, ALU.mult)
        k.tt("vector", sm["msq"][:], sm["mean"][:], sm["mean"][:], ALU.mult)
        k.stt("vector", sm["var"][:], sm["s2"][:], 1.0 / 64, sm["msq"][:], ALU.mult, ALU.subtract)
        k.ts("vector", sm["var"][:], sm["var"][:], 1e-5, None, ALU.add)
        k.act(sm["sd"][:], sm["var"][:], AF.Sqrt)
        recip(k, "vector", sm["rstd"][:], sm["sd"][:])
        k.stt("vector", sm["nmr"][:], sm["mean"][:], -1.0, sm["rstd"][:], ALU.mult, ALU.mult)
        k.act(ki[:], kvb[:, 128:192], AF.Identity, bias=sm["nmr"][:], scale=sm["rstd"][:])
        k.tt("vector", ki[:], ki[:], gki[:], ALU.mult)
        k.tt("vector", ki[:], ki[:], bki[:], ALU.add)
        k.transpose(pt_[0:64, 128:256], ki[:], ident[:])
        k.copy("vector", kidxT[:, ti * 128:(ti + 1) * 128], pt_[0:64, 128:256])

    out_ops = []
    ctr = {"ri": 0, "xi": 0}
    qlatT2 = [qlatT, k.sbuf("qlatT_b", [128, 8, 128], BF16)]
    sm["sa"] = k.sbuf("sm_sa", [128, 1], F32)
    sm["nmid"] = k.sbuf("sm_nmid", [128, 1], F32)
    OB = [PB[4], PB[5], PB[6]]
    hb = [(0, 0), (0, 1), (0, 2), (1, 0), (1, 1), (1, 2), (2, 0), (2, 1)]

    def phase_Q(slot):
        xt = xtb[slot % 2]
        qlt = qlatT2[slot % 2]
        k.dma("gpsimd", xt[:], xqT_d.rearrange("(c p) t -> p c t", p=128)[:, :, slot * 128:(slot + 1) * 128])
        pq = PB[0]
        for c in range(8):
            k.matmul(pq[:, 0:264], xt[:, c, :], wq[:, c, :], start=(c == 0), stop=(c == 7))
        k.copy("scalar", cq[:], pq[:, 0:264])
        rms_stats(cq[:, 0:256], 256, 1e-6)
        k.stt("vector", cqn[:], cq[:, 0:256], sm["rstd"][:], gq[:], ALU.mult, ALU.mult)
        ptq = PB[1]
        for rc in range(2):
            k.transpose(ptq[:, rc * 128:(rc + 1) * 128], cqn[:, rc * 128:(rc + 1) * 128], ident[:])
        src = ptq.v(ptq.ap[:, 0:256].rearrange("p (c m) -> p c m", c=2))
        k.copy("vector", cqT32[:], src)
        k.copy("scalar", cqTb[:], cqT32[:])
        for hg in range(2):
            pqi = PB[2 + hg]
            for h4 in range(4):
                h = hg * 4 + h4
                for rc in range(2):
                    k.matmul(pqi[0:64, h4 * 128:(h4 + 1) * 128], wqidx[:, rc, h * 64:(h + 1) * 64], cqT32[:, rc, :],
                             start=(rc == 0), stop=(rc == 1))
            k.copy("vector", qidxT[:, hg * 4:(hg + 1) * 4, :], pqi.v(pqi.ap[0:64, :].rearrange("p (h q) -> p h q", h=4)))
        for hg in range(2):
            pql = PB[hg]
            for h4 in range(4):
                h = hg * 4 + h4
                for rc in range(2):
                    k.matmul(pql[:, h4 * 128:(h4 + 1) * 128], wqlat[:, rc, h, :], cqTb[:, rc, :],
                             start=(rc == 0), stop=(rc == 1))
            k.copy("scalar", qlt[:, hg * 4:(hg + 1) * 4, :], pql.v(pql.ap.rearrange("p (h q) -> p h q", h=4)))

    def gen_I(slot):
        NK = dsa_nb(slot) * 128
        nch = (NK + 511) // 512
        for kc in range(nch):
            k0 = kc * 512
            n = min(512, NK - k0)
            for h in range(8):
                ps_ = PB[ctr["ri"] % 2]
                rt = rl[ctr["ri"] % 3]
                ctr["ri"] += 1
                k.matmul(ps_[:, 0:n], qidxT[:, h, :], kidxT[:, k0:k0 + n], start=True, stop=True)
                k.act(rt[:, 0:n], ps_[:, 0:n], AF.Relu)
                if h == 0:
                    k.ts("vector", isc[:, k0:k0 + n], rt[:, 0:n], cq[:, 256:257], None, ALU.mult)
                else:
                    k.stt("vector", isc[:, k0:k0 + n], rt[:, 0:n], cq[:, 256 + h:257 + h], isc[:, k0:k0 + n], ALU.mult, ALU.add)
                yield

    def phase_B(slot):
        NK = dsa_nb(slot) * 128
        hi_slot = slot % 2
        half = (NK // 256) * 128
        n2 = NK - half
        k.reduce("vector", sm["lo"][:], isc[:, 0:NK], ALU.min)
        k.reduce("vector", sm["hi"][:], isc[:, 0:NK], ALU.max)
        k.tt("vector", isc[:, NK - 512:NK], isc[:, NK - 512:NK], cb[:, hi_slot, :], ALU.add)
        for it in range(NIT):
            k.tt("vector", sm["mid"][:], sm["lo"][:], sm["hi"][:], ALU.add)
            k.ts("vector", sm["mid"][:], sm["mid"][:], 0.5, None, ALU.mult)
            k.ts("vector", sm["nmid"][:], sm["mid"][:], -1.0, None, ALU.mult)
            k.act(mask_alias[:, half:NK], isc[:, half:NK], AF.Sign, bias=sm["nmid"][:], accum_out=sm["sa"][:], extra_reads=[maskT])
            k.ts("vector", mask[:, 0:half], isc[:, 0:half], sm["mid"][:], None, ALU.is_ge, ALU.add, accum_out=sm["cnt"][:])
            k.stt("vector", sm["cnt"][:], sm["sa"][:], 0.5, sm["cnt"][:], ALU.mult, ALU.add)
            k.ts("vector", sm["ge"][:], sm["cnt"][:], float(TOPK) - 0.5 - 0.5 * n2, None, ALU.is_ge)
            k.tt("vector", sm["d1"][:], sm["mid"][:], sm["lo"][:], ALU.subtract)
            k.tt("vector", sm["d2"][:], sm["hi"][:], sm["mid"][:], ALU.subtract)
            k.stt("vector", sm["lo"][:], sm["d1"][:], sm["ge"][:], sm["lo"][:], ALU.mult, ALU.add)
            k.stt("vector", sm["hi"][:], sm["d2"][:], sm["ge"][:], sm["mid"][:], ALU.mult, ALU.add)

    def phase_M(slot):
        NB = dsa_nb(slot)
        NK = NB * 128
        k.ts("vector", mask[:, 0:NK], isc[:, 0:NK], sm["lo"][:], None, ALU.is_ge)
        for kb8 in range((NB + 7) // 8):
            nb_ = min(8, NB - kb8 * 8)
            for j in range(nb_):
                kb = kb8 * 8 + j
                k.transpose(PBb[:, j * 128:(j + 1) * 128], mask[:, kb * 128:(kb + 1) * 128], identb[:])
            k.copy("scalar" if kb8 % 2 else "vector", maskT[:, kb8 * 8:kb8 * 8 + nb_, :],
                   PBb.v(PBb.ap[:, 0:nb_ * 128].rearrange("p (b q) -> p b q", b=nb_)))

    def gen_A(slot):
        NB = dsa_nb(slot)
        qlt = qlatT2[slot % 2]
        started = [False, False, False]
        pend = []

        def emit_pv(ptt, kb, hg):
            for h4 in range(4):
                h = hg * 4 + h4
                bnk, sl = hb[h]
                first = not started[bnk]
                started[bnk] = True
                k.matmul(OB[bnk][:, sl * 129:(sl + 1) * 129], ptt[:, h4, :], ckvx[:, kb, :], start=first,
                         stop=(kb == NB - 1), skip_group_check=True)
        for kb in range(NB):
            for hg in range(2):
                ps_ = PB[2 + ctr["xi"] % 2]
                ext = ex[ctr["xi"] % 3]; ptt = pT[ctr["xi"] % 3]
                ctr["xi"] += 1
                k.matmul(ps_[:], ckvT[:, kb * 128:(kb + 1) * 128],
                         qlt.v(qlt.ap[:, hg * 4:(hg + 1) * 4, :].rearrange("p h q -> p (h q)")), start=True, stop=True)
                k.act(ext.v(ext.ap.rearrange("p h q -> p (h q)")), ps_[:], AF.Exp, scale=0.125)
                mb = maskT.v(maskT.ap[:, kb, :].unsqueeze(1).to_broadcast([128, 4, 128]))
                k.tt("gpsimd", ptt[:], ext[:], mb, ALU.mult)
                pend.append((ptt, kb, hg))
                if len(pend) > 2:
                    emit_pv(*pend.pop(0))
                yield
        while pend:
            emit_pv(*pend.pop(0))
        yield

    def phase_F(slot):
        for h in range(8):
            bnk, sl = hb[h]
            recip(k, "vector", rd8[:, h, :], OB[bnk][:, sl * 129 + 128:sl * 129 + 129])
            k.ts("vector", oln[:, h, :], OB[bnk][:, sl * 129:sl * 129 + 128], rd8[:, h, :], None, ALU.mult)
        for hg in range(2):
            pt_ = PB[hg]
            for h4 in range(4):
                k.transpose(pt_[:, h4 * 128:(h4 + 1) * 128], oln[:, hg * 4 + h4, :], ident[:])
            k.copy("vector" if hg == 0 else "scalar", olT[:, hg * 4:(hg + 1) * 4, :],
                   pt_.v(pt_.ap.rearrange("p (h q) -> p h q", h=4)))
        po = PB[2]
        for h in range(8):
            k.matmul(po[:, h * 64:(h + 1) * 64], olT[:, h, :], wuv[:, h * 64:(h + 1) * 64], start=True, stop=True)
        oab = oa[slot % 2]
        k.copy("vector", oab[:], po[:])
        out_ops.append(k.dma("sync", out_d[slot * 128:(slot + 1) * 128, :], oab[:]))

    phase_Q(0)
    for _ in gen_I(0):
        pass
    phase_B(0)
    phase_M(0)
    for slot in range(NSLOT):
        gi = iter(())
        if slot + 1 < NSLOT:
            phase_Q(slot + 1)
            gi = gen_I(slot + 1)
        ga = gen_A(slot)
        done_i = done_a = False
        while not (done_i and done_a):
            if not done_i:
                try:
                    next(gi)
                except StopIteration:
                    done_i = True
            if not done_a:
                try:
                    next(ga)
                except StopIteration:
                    done_a = True
        phase_F(slot)
        if slot + 1 < NSLOT:
            phase_B(slot + 1)
            phase_M(slot + 1)
    if standalone:
        k.finish(out_ops)
        return nc
    k.end_stage()
    return out_ops

import math


def recip(k, eng, out, in_):
    o, i = _ap(out), _ap(in_)
    return k.op(eng, lambda e: e.reciprocal(o, i), [in_], [out])


def gdn_consts():
    i = np.arange(128)[:, None]; j = np.arange(128)[None, :]
    U = (i <= j).astype(np.float32)
    L = (j <= i).astype(np.float32)
    sbn = -((j < i) & ((i // 64) == (j // 64))).astype(np.float32)
    off = ((i >= 64) & (j < 64)).astype(np.float32)
    return np.ascontiguousarray(np.stack([U, L, sbn, off, np.eye(128, dtype=np.float32), np.ones((128, 128), np.float32)], 1))


def build_gdn(S=8192, k=None, dm=None, pfx=""):
    standalone = k is None
    if standalone:
        k = KB(bass.Bass("TRN2", target_bir_lowering=False))
    nc = k.nc
    dm = dm or {}
    NG = S // 512

    def din(name, shape):
        if name in dm:
            return dm[name]
        return nc.dram_tensor(pfx + name, list(shape), F32, kind="ExternalInput").ap()
    xT_d = din("xT", [1024, S])
    wqkv_d = din("wqkv", [1024, 3, 128])
    wz_d = din("wz", [1024, 130])
    cw_d = din("cw", [128, 3, 4])
    hp_d = din("hp", [128, 2])
    ng_d = din("ng", [1, 128])
    cst_d = din("cst", [128, 6, 128])
    out_d = dm["out"] if "out" in dm else nc.dram_tensor("out", [S, 128], F32, kind="ExternalOutput").ap()

    if not standalone:
        k.begin_stage(pfx)
    cst = k.sbuf("cst", [128, 6, 128], F32)
    U, L, SBN, OFF, I_, ONES = (cst[:, i, :] for i in range(6))
    wqkv = k.sbuf("wqkv", [128, 8, 3, 128], BF16)
    wz = k.sbuf("wz", [128, 8, 130], BF16)
    cw = k.sbuf("cw", [128, 3, 4], F32)
    hp = k.sbuf("hp", [128, 2], F32)
    ng = k.sbuf("ng", [128, 128], F32)
    nexpal = k.sbuf("nexpal", [128, 1], F32)
    epsq = k.sbuf("epsq", [128, 1], F32)
    lnq = k.sbuf("lnq", [128, 1], F32)
    zero1 = k.sbuf("zero1", [128, 1], F32)
    xg = [k.sbuf(f"xg{i}", [128, 8, 512], BF16) for i in range(2)]
    cbuf = [k.sbuf(f"cbuf{i}", [128, 515], F32) for i in range(3)]
    ycv = k.sbuf("ycv", [128, 512], F32)
    ys = [k.sbuf(f"ys{i}", [128, 512], F32) for i in range(3)]
    sqb = k.sbuf("sqb", [128, 512], F32)
    lnv = k.sbuf("lnv", [128, 512], F32)
    qnT = k.sbuf("qnT", [128, 512], F32)
    knT = k.sbuf("knT", [128, 512], F32)
    S_ = k.sbuf("S", [128, 128], F32)

    def tset(p):
        names = ["D", "t1", "Ya", "Yb", "Xa", "Xb", "Pa", "Pb", "Qa", "Qb", "Aoff", "Z", "TT", "qkm", "qkmT",
                 "ktok", "vtok", "kbg", "vb", "kdec", "negwT", "vnew", "t3", "o", "ngb", "Gm", "zs", "on"]
        d = {n: k.sbuf(f"{n}{p}", [128, 128], F32) for n in names}
        d["zba"] = k.sbuf(f"zba{p}", [128, 130], F32)
        for n in ["beta", "g", "gc", "gl", "egc", "edec", "egl", "sp", "s2", "var", "sd", "rstd", "dg"]:
            d[n] = k.sbuf(f"v_{n}{p}", [128, 1], F32)
        return d
    TS = [tset(0), tset(1), tset(2), tset(3)]
    NPB = 8
    PB = [k.psum(f"pb{i}", [128, 512], F32) for i in range(NPB)]
    pctr = [0]

    def pnext():
        p = PB[2 + pctr[0] % (NPB - 2)]
        pctr[0] += 1
        return p

    k.dma("sync", cst[:], cst_d)
    k.dma("gpsimd", wqkv[:], wqkv_d.rearrange("(c p) x n -> p c x n", p=128))
    k.dma("gpsimd", wz[:], wz_d.rearrange("(c p) n -> p c n", p=128))
    k.dma("sync", cw[:], cw_d); k.dma("sync", hp[:], hp_d)
    k.dma("sync", ng[:], ng_d.partition_broadcast(128))
    k.act(nexpal[:], hp[:, 0:1], AF.Exp)
    k.ts("vector", nexpal[:], nexpal[:], -1.0, None, ALU.mult)
    k.memset("vector", epsq[:], 1e-6)
    k.memset("vector", lnq[:], -0.5 * math.log(128.0))
    k.memset("vector", zero1[:], 0.0)
    k.memset("vector", S_[:], 0.0)
    for i in range(3):
        k.memset("vector", cbuf[i][:, 0:3], 0.0)

    out_ops = []
    evi = [0]

    def evac(dst, src, scale=None):
        e = "scalar" if evi[0] % 2 else "vector"
        evi[0] += 1
        if scale is None:
            k.copy(e, dst, src)
        elif e == "scalar":
            k.act(dst, src, AF.Identity, scale=scale)
        else:
            k.ts("vector", dst, src, scale, None, ALU.mult)

    for g in range(NG):
        xgt = xg[g % 2]
        k.dma("gpsimd", xgt[:], xT_d.rearrange("(c p) t -> p c t", p=128)[:, :, g * 512:(g + 1) * 512])
        for x in range(3):
            pj = PB[x % 2]
            for c in range(8):
                k.matmul(pj[:], wqkv[:, c, x, :], xgt[:, c, :], start=(c == 0), stop=(c == 7))
            cb_ = cbuf[x]
            if g > 0:
                k.copy("vector", cb_[:, 0:3], cb_[:, 512:515])
            k.copy("scalar", cb_[:, 3:515], pj[:])
            k.ts("vector", ycv[:], cb_[:, 0:512], cw[:, x, 0:1], None, ALU.mult)
            for j in range(1, 4):
                k.stt("vector", ycv[:], cb_[:, j:j + 512], cw[:, x, j:j + 1], ycv[:], ALU.mult, ALU.add)
            k.act(ys[x][:], ycv[:], AF.Silu)
        for x, dst in ((0, qnT), (1, knT)):
            k.act(sqb[:], ys[x][:], AF.Square)
            pn = PB[x % 2]
            k.matmul(pn[:], ONES, sqb[:], start=True, stop=True)
            k.act(lnv[:], pn[:], AF.Ln, bias=epsq[:])
            k.act(lnv[:], lnv[:], AF.Exp, scale=-0.5, bias=(lnq[:] if x == 0 else zero1[:]))
            k.tt("vector", dst[:], ys[x][:], lnv[:], ALU.mult)
        def P_gen(tt_):
            ti = g * 4 + tt_
            T = TS[ti % 4]
            tl = slice(tt_ * 128, (tt_ + 1) * 128)
            yield
            pz = pnext()
            for c in range(8):
                k.matmul(pz[:, 0:130], xgt[:, c, tl], wz[:, c, :], start=(c == 0), stop=(c == 7))
            k.copy("vector", T["zba"][:], pz[:, 0:130])
            yield
            p1 = pnext()
            k.transpose(p1[:, 0:128], knT[:, tl], I_)
            evac(T["ktok"][:], p1[:, 0:128])
            yield
            p2 = pnext()
            k.transpose(p2[:, 0:128], ys[2][:, tl], I_)
            evac(T["vtok"][:], p2[:, 0:128])
            k.act(T["beta"][:], T["zba"][:, 128:129], AF.Sigmoid)
            k.act(T["sp"][:], T["zba"][:, 129:130], AF.Exp, bias=hp[:, 1:2])
            k.ts("vector", T["sp"][:], T["sp"][:], 1.0, None, ALU.add)
            k.act(T["sp"][:], T["sp"][:], AF.Ln)
            k.tt("vector", T["g"][:], T["sp"][:], nexpal[:], ALU.mult)
            yield
            p3 = pnext()
            k.matmul(p3[:, 0:1], U, T["g"][:], start=True, stop=True)
            k.matmul(p3[:, 1:2], ONES, T["g"][:], start=True, stop=True)
            k.copy("vector", T["gc"][:], p3[:, 0:1])
            k.copy("vector", T["gl"][:], p3[:, 1:2])
            k.act(T["egc"][:], T["gc"][:], AF.Exp)
            k.act(T["egl"][:], T["gl"][:], AF.Exp)
            k.tt("vector", T["dg"][:], T["gl"][:], T["gc"][:], ALU.subtract)
            k.act(T["edec"][:], T["dg"][:], AF.Exp)
            k.ts("vector", T["ngb"][:], ONES, T["g"][:], -1.0, ALU.mult, ALU.mult)
            yield
            p4 = pnext()
            k.matmul(p4[:, 0:128], T["ngb"][:], U, start=True, stop=True)
            k.ts("vector", T["Gm"][:], p4[:, 0:128], T["gc"][:], 0.0, ALU.add, ALU.min)
            k.act(T["D"][:], T["Gm"][:], AF.Exp)
            yield
            p5 = pnext()
            k.matmul(p5[:, 0:128], knT[:, tl], knT[:, tl], start=True, stop=True)
            k.tt("vector", T["t1"][:], p5[:, 0:128], T["D"][:], ALU.mult)
            k.stt("vector", T["Ya"][:], T["t1"][:], T["beta"][:], SBN, ALU.mult, ALU.mult)
            k.stt("vector", T["Aoff"][:], T["t1"][:], T["beta"][:], OFF, ALU.mult, ALU.mult)
            yield
            p6 = pnext()
            k.matmul(p6[:, 0:128], qnT[:, tl], knT[:, tl], start=True, stop=True)
            k.tt("vector", T["qkm"][:], p6[:, 0:128], T["D"][:], ALU.mult)
            k.tt("gpsimd", T["qkm"][:], T["qkm"][:], L, ALU.mult)
            yield
            p7 = pnext()
            k.transpose(p7[:, 0:128], T["qkm"][:], I_)
            evac(T["qkmT"][:], p7[:, 0:128])
            yield
            p8 = pnext()
            k.transpose(p8[:, 0:128], T["Ya"][:], I_)
            evac(T["Xa"][:], p8[:, 0:128])
            k.tt("gpsimd", T["Pa"][:], T["Xa"][:], I_, ALU.add)
            k.tt("gpsimd", T["Qa"][:], T["Ya"][:], I_, ALU.add)
            cur, nxt = "a", "b"
            for lvl in range(1, 6):
                Xc, Yc, Pc, Qc = T["X" + cur], T["Y" + cur], T["P" + cur], T["Q" + cur]
                Xn, Yn, Pn, Qn = T["X" + nxt], T["Y" + nxt], T["P" + nxt], T["Q" + nxt]
                yield
                pa = pnext()
                k.matmul(pa[:, 0:128], Yc[:], Xc[:], start=True, stop=True)
                evac(Xn[:], pa[:, 0:128])
                if lvl < 5:
                    yield
                    pb_ = pnext()
                    k.matmul(pb_[:, 0:128], Xc[:], Yc[:], start=True, stop=True)
                    evac(Yn[:], pb_[:, 0:128])
                yield
                pc = pnext()
                k.matmul(pc[:, 0:128], Qc[:], Xn[:], start=True, stop=True)
                k.tt("vector", Pn[:], pc[:, 0:128], Pc[:], ALU.add)
                yield
                pd = pnext()
                k.matmul(pd[:, 0:128], Xn[:], Qc[:], start=True, stop=True)
                k.tt("vector", Qn[:], pd[:, 0:128], Qc[:], ALU.add)
                cur, nxt = nxt, cur
            P5, Q5 = T["P" + cur], T["Q" + cur]
            yield
            pe = pnext()
            k.matmul(pe[:, 0:128], T["Aoff"][:], P5[:], start=True, stop=True)
            evac(T["Z"][:], pe[:, 0:128])
            yield
            pf = pnext()
            k.matmul(pf[:, 0:128], Q5[:], T["Z"][:], start=True, stop=True)
            k.tt("vector", T["TT"][:], P5[:], pf[:, 0:128], ALU.subtract)
            k.ts("vector", T["kbg"][:], T["ktok"][:], T["beta"][:], T["egc"][:], ALU.mult, ALU.mult)
            k.ts("gpsimd", T["vb"][:], T["vtok"][:], T["beta"][:], None, ALU.mult)
            k.ts("gpsimd", T["kdec"][:], T["ktok"][:], T["edec"][:], None, ALU.mult)
            yield
            pg_ = pnext()
            k.matmul(pg_[:, 0:128], T["kbg"][:], T["TT"][:], start=True, stop=True)
            evac(T["negwT"][:], pg_[:, 0:128], scale=-1.0)

            yield
        def R_fn(tt_):
            ti = g * 4 + tt_
            T = TS[ti % 4]
            tl = slice(tt_ * 128, (tt_ + 1) * 128)
            ph = PB[0]
            k.matmul(ph[:, 0:128], T["TT"][:], T["vb"][:], start=True, stop=False)
            k.matmul(ph[:, 0:128], T["negwT"][:], S_[:], start=False, stop=True)
            k.matmul(ph[:, 128:256], qnT[:, tl], S_[:], start=True, stop=True)
            k.copy("vector", T["vnew"][:], ph[:, 0:128])
            k.act(T["t3"][:], ph[:, 128:256], AF.Identity, scale=T["egc"][:])
            pi_ = PB[1]
            k.matmul(pi_[:, 0:128], T["kdec"][:], T["vnew"][:], start=True, stop=True)
            k.matmul(pi_[:, 128:256], T["qkmT"][:], T["vnew"][:], start=True, stop=True)
            k.stt("vector", S_[:], S_[:], T["egl"][:], pi_[:, 0:128], ALU.mult, ALU.add)
            k.tt("vector", T["o"][:], T["t3"][:], pi_[:, 128:256], ALU.add)
            k.act(T["zs"][:], T["o"][:], AF.Square, accum_out=T["s2"][:])
            k.ts("vector", T["var"][:], T["s2"][:], 1.0 / 128, 1e-6, ALU.mult, ALU.add)
            k.act(T["sd"][:], T["var"][:], AF.Sqrt)
            recip(k, "vector", T["rstd"][:], T["sd"][:])
            k.stt("vector", T["on"][:], T["o"][:], T["rstd"][:], ng[:], ALU.mult, ALU.mult)
            k.act(T["zs"][:], T["zba"][:, 0:128], AF.Silu)
            k.tt("gpsimd", T["on"][:], T["on"][:], T["zs"][:], ALU.mult)
            out_ops.append(k.dma("sync", out_d[ti * 128:(ti + 1) * 128, :], T["on"][:]))

        alive = [P_gen(0), P_gen(1), P_gen(2), P_gen(3)]
        while alive:
            for ge in list(alive):
                try:
                    next(ge)
                except StopIteration:
                    alive.remove(ge)
        for tt_ in range(4):
            R_fn(tt_)
    if standalone:
        k.finish(out_ops)
        return nc
    k.end_stage()
    return out_ops


def build_mix(NTOK=2048):
    nc = bass.Bass("TRN2", target_bir_lowering=False)
    D = 1024
    NT = NTOK // 128

    def din(name, shape):
        return nc.dram_tensor(name, list(shape), F32, kind="ExternalInput").ap()
    oT_d = din("oT", [D, NTOK]); x_d = din("x", [NTOK, D]); wout_d = din("w_out", [D, D])
    lng_d = din("lng", [1, D]); lnb_d = din("lnb", [1, D])
    out_d = nc.dram_tensor("out", [NTOK, D], F32, kind="ExternalOutput").ap()
    k = KB(nc)
    oT_all = k.sbuf("oT", [128, 8, NTOK], BF16)
    oT = [Tile(oT_all.ap[:, :, g * 512:(g + 1) * 512], f"oT{g}") for g in range(NTOK // 512)]
    wout = k.sbuf("wout", [128, 8, D], BF16)
    lng_s = k.sbuf("lng_s", [128, D], F32); lnb_s = k.sbuf("lnb_s", [128, D], F32)
    hs = [k.sbuf(f"hs{i}", [128, D], F32) for i in range(2)]
    acc = [k.sbuf(f"acc{i}", [128, D], F32) for i in range(2)]
    scr = k.sbuf("scr", [128, D], F32)
    small = {n: k.sbuf("sm_" + n, [128, 1], F32) for n in ("s1", "s2", "mean", "msq", "var", "sd", "rstd", "nmr")}
    PB = [k.psum(f"pb{i}", [128, 512], F32) for i in range(4)]
    k.dma("gpsimd", wout[:], wout_d.rearrange("(c p) n -> p c n", p=128))
    for g in range(NTOK // 512):
        k.dma("gpsimd", oT[g][:], oT_d.rearrange("(c p) t -> p c t", p=128)[:, :, g * 512:(g + 1) * 512])
    k.dma("sync", lng_s[:], lng_d.partition_broadcast(128)); k.dma("sync", lnb_s[:], lnb_d.partition_broadcast(128))
    out_ops = []
    for t in range(NT):
        hsb = hs[t % 2]; ac = acc[t % 2]
        g = t // 4
        tl = slice((t % 4) * 128, (t % 4 + 1) * 128)
        k.dma("sync", hsb[:], x_d[t * 128:(t + 1) * 128, :])
        for half in range(2):
            hsl = slice(half * 512, (half + 1) * 512)
            pm = PB[(t * 2 + half) % 4]
            for c in range(8):
                k.matmul(pm[:], oT[g][:, c, tl], wout[:, c, hsl], start=(c == 0), stop=(c == 7))
            k.stt("vector", ac[:, hsl], hsb[:, hsl], ALPHA_, pm[:], ALU.mult, ALU.add)
        ln_rows(k, ac[:], hsb[:], scr, small, lng_s, lnb_s, 1e-5)
        out_ops.append(k.dma("sync", out_d[t * 128:(t + 1) * 128, :], hsb[:]))
    k.finish(out_ops)
    return nc


NCORES = 8
ALLG = [list(range(NCORES))]
NG4 = 8


def emit_mixsel(k, OA_all, OB_all, HM0, ident_d, pfx="x_"):
    nc = k.nc
    D = 1024

    def din(name, shape):
        return nc.dram_tensor(pfx + name, list(shape), F32, kind="ExternalInput").ap()
    x_d = din("x", [2048, D]); wout_d = din("w_out", [D, D]); lng_d = din("lng", [1, D]); lnb_d = din("lnb", [1, D])
    sel_d = din("sel", [128, 8])
    k.begin_stage(pfx)
    ident = k.sbuf("ident", [128, 128], F32)
    sel = k.sbuf("sel", [128, 8], F32)
    wout = k.sbuf("wout", [128, 8, D], BF16)
    lng_s = k.sbuf("lng_s", [128, D], F32); lnb_s = k.sbuf("lnb_s", [128, D], F32)
    ca = [k.sbuf(f"ca{i}", [128, 8, 512], F32) for i in range(2)]
    cbb = [k.sbuf(f"cbb{i}", [128, 8, 512], F32) for i in range(2)]
    oab = [k.sbuf(f"oab{i}", [128, D], F32) for i in range(2)]
    oT = [k.sbuf(f"oT{i}", [128, 8, 128], BF16) for i in range(2)]
    hs = [k.sbuf(f"hs{i}", [128, D], F32) for i in range(2)]
    acc = k.sbuf("acc", [128, D], F32)
    scr = k.sbuf("scr", [128, D], F32)
    small = {n: k.sbuf("sm_" + n, [128, 1], F32) for n in ("s1", "s2", "mean", "msq", "var", "sd", "rstd", "nmr")}
    PB = [k.psum(f"pb{i}", [128, 512], F32) for i in range(4)]
    k.dma("sync", ident[:], ident_d)
    k.dma("sync", sel[:], sel_d)
    k.dma("gpsimd", wout[:], wout_d.rearrange("(c p) n -> p c n", p=128))
    k.dma("sync", lng_s[:], lng_d.partition_broadcast(128)); k.dma("sync", lnb_s[:], lnb_d.partition_broadcast(128))
    oa6 = OA_all.rearrange("(b r q s p) c -> p b r q s c", b=2, r=4, q=4, s=4, p=128)
    ob6 = OB_all.rearrange("(b h q t p) c -> p b q h t c", b=2, h=4, q=4, t=16, p=128)
    for lt in range(16):
        w = lt % 8
        hi = 1 if w >= 4 else 0
        r = w if w < 4 else 7 - w
        sidx = 2 * (lt // 8) + hi
        cat = ca[lt % 2]; cbt = cbb[lt % 2]; oabt = oab[lt % 2]; oTt = oT[lt % 2]; hsb = hs[lt % 2]
        for b_ in range(2):
            k.dma("sync", cat[:, b_ * 4:(b_ + 1) * 4, :], oa6[:, b_, r, :, sidx, :])
            for q_ in range(4):
                k.dma("sync", cbt.v(cbt.ap[:, b_ * 4 + q_, :].rearrange("p (h d) -> p h d", h=4)), ob6[:, b_, q_, :, lt, :])
        k.dma("sync", hsb[:], x_d[lt * 128:(lt + 1) * 128, :])
        for src, cols in ((cat, slice(0, 512)), (cbt, slice(512, 1024))):
            k.ts("vector", oabt[:, cols], src[:, 0, :], sel[:, 0:1], None, ALU.mult)
            for cc in range(1, 8):
                k.stt("vector", oabt[:, cols], src[:, cc, :], sel[:, cc:cc + 1], oabt[:, cols], ALU.mult, ALU.add)
        for c in range(8):
            k.transpose(PB[c // 4][:, (c % 4) * 128:(c % 4 + 1) * 128], oabt[:, c * 128:(c + 1) * 128], ident[:])
        for hh in range(2):
            k.copy("vector" if hh == 0 else "scalar", oTt[:, hh * 4:(hh + 1) * 4, :],
                   PB[hh].v(PB[hh].ap.rearrange("p (c m) -> p c m", c=4)))
        for half in range(2):
            hsl = slice(half * 512, (half + 1) * 512)
            pm = PB[2 + half]
            for c in range(8):
                k.matmul(pm[:], oTt[:, c, :], wout[:, c, hsl], start=(c == 0), stop=(c == 7))
            k.stt("vector", acc[:, hsl], hsb[:, hsl], ALPHA_, pm[:], ALU.mult, ALU.add)
        ln_rows(k, acc[:], hsb[:], scr, small, lng_s, lnb_s, 1e-5)
        k.dma("sync", HM0[lt * 128:(lt + 1) * 128, :], hsb[:])
    k.end_stage()


def emit_halo(k, H1_all, H1_loc, HX, pfx="h_"):
    nc = k.nc
    selp_d = nc.dram_tensor(pfx + "selp", [128, 8], F32, kind="ExternalInput").ap()
    k.begin_stage(pfx)
    selp = k.sbuf("selp", [128, 8], F32)
    cand = [k.sbuf(f"cand{i}", [128, 4, 1024], F32) for i in range(2)]
    acc = [k.sbuf(f"acc{i}", [128, 1024], F32) for i in range(2)]
    k.dma("sync", selp[:], selp_d)
    k.dma("sync", HX[2048:4096, :], H1_loc)
    h4 = H1_all.rearrange("(c t p) d -> p c t d", c=8, t=16, p=128)
    ci = 0
    for ti in range(16):
        ac = acc[ti % 2]
        for piece in range(2):
            cd_ = cand[ci % 2]
            ci += 1
            k.dma("sync", cd_[:], h4[:, piece * 4:(piece + 1) * 4, ti, :])
            for j in range(4):
                cc = piece * 4 + j
                if cc == 0:
                    k.ts("vector", ac[:], cd_[:, j, :], selp[:, 0:1], None, ALU.mult)
                else:
                    k.stt("vector", ac[:], cd_[:, j, :], selp[:, cc:cc + 1], ac[:], ALU.mult, ALU.add)
        k.dma("sync", HX[ti * 128:(ti + 1) * 128, :], ac[:])
    k.end_stage()


def build_fused():
    nc = bass.Bass("TRN2", target_bir_lowering=False)
    k = KB(nc)

    def ext(name, shape):
        return nc.dram_tensor(name, list(shape), F32, kind="ExternalInput").ap()

    def scratch(name, shape):
        return nc.dram_tensor(name, list(shape), F32).ap()
    ident = ext("ident", [128, 128])
    xT = ext("xT", [1024, 8192])
    OA_loc = scratch("OA_loc", [2048, 512]); OB_loc = scratch("OB_loc", [8192, 128])
    OA_all = scratch("OA_all", [NG4 * 2048, 512]); OB_all = scratch("OB_all", [NG4 * 8192, 128])
    HM0 = scratch("HM0", [2048, 1024]); H1_loc = scratch("H1_loc", [2048, 1024])
    H1_all = scratch("H1_all", [NG4 * 2048, 1024]); HX = scratch("HX", [4096, 1024]); HM1 = scratch("HM1", [2048, 1024])
    out = nc.dram_tensor("out", [2048, 1024], F32, kind="ExternalOutput").ap()

    build_dsa(k=k, dm={"xT": xT, "ident": ident, "out": OA_loc}, pfx="a_")
    build_gdn(k=k, dm={"xT": xT, "out": OB_loc}, pfx="g_")
    k.collective("AllGather", OA_all, OA_loc, ALLG)
    k.collective("AllGather", OB_all, OB_loc, ALLG)
    k.barrier()
    emit_mixsel(k, OA_all, OB_all, HM0, ident)
    build_moe(k=k, dm={"h": HM0, "ident": ident, "out": H1_loc}, pfx="m0_")
    k.collective("AllGather", H1_all, H1_loc, ALLG)
    k.barrier()
    emit_halo(k, H1_all, H1_loc, HX)
    build_cd(k=k, dm={"hx": HX, "ident": ident, "out": HM1}, pfx="c_")
    outs = build_moe(k=k, dm={"h": HM1, "ident": ident, "out": out}, pfx="m1_")
    k.finish(outs)
    return nc


NCORES = 8
_C = np.ascontiguousarray


def _run(nc, in_maps):
    res = run_bass_kernel_spmd(nc, in_maps, core_ids=list(range(NCORES)))
    return [r["out"] for r in res.results]


def moe_inputs(inp, L, h, pT):
    wgu = inp["w_gu"][L]
    bgu = inp["b_gu"][L]

    def blay(b):
        return _C(b.reshape(32, 8, 128).transpose(2, 0, 1))
    return {
        "h": _C(h), "pT": _C(pT),
        "rw": _C(inp["router_w"][L]), "rb": _C(inp["router_b"][L][None]),
        "wg": _C(wgu[:, :, 0::2]), "wu": _C(wgu[:, :, 1::2]),
        "bg": blay(bgu[:, 0::2]), "bu": blay(bgu[:, 1::2]),
        "wd": _C(inp["w_down"][L]), "bd": _C(inp["b_down"][L]),
        "pwp": _C(inp["ple_w_proj"][L]), "pwg": _C(inp["ple_w_gate"][L]),
        "lng": _C(inp["ln_ffn_g"][L][None]), "lnb": _C(inp["ln_ffn_b"][L][None]),
        "ident": np.eye(128, dtype=np.float32),
    }


def cd_inputs(inp, h1_b, kq, NTOK=2048, HALO=2048):
    s0 = kq * NTOK
    hx = np.zeros((HALO + NTOK, 1024), np.float32)
    hx[HALO:] = h1_b[s0:s0 + NTOK]
    if kq > 0:
        hx[:HALO] = h1_b[s0 - HALO:s0]
    wpart = np.zeros((128, 2), np.float32)
    wpart[0:64, 0] = 2; wpart[64:128, 0] = 4; wpart[0:64, 1] = 8; wpart[64:128, 1] = 16
    t = np.arange(16, dtype=np.float32)[None, None, :]
    if kq == 0:
        invcnt = 1.0 / np.minimum(t + 1, wpart[:, :, None])
    else:
        invcnt = np.broadcast_to(1.0 / wpart[:, :, None], (128, 2, 16))
    return {
        "hx": hx, "w_in": _C(inp["cd_w_in"][0]), "pool_w": _C(inp["cd_pool_w"][0]),
        "pool_scale": _C(inp["cd_pool_scale"][0].reshape(2, 128).T),
        "w_out": _C(inp["cd_w_out"][0]),
        "lng": _C(inp["ln_mix_g"][1][None]), "lnb": _C(inp["ln_mix_b"][1][None]),
        "ident": np.eye(128, dtype=np.float32), "masks": cd_masks(),
        "valid": np.full((128, 1), 1.0 if kq > 0 else 0.0, np.float32),
        "invcnt": _C(invcnt.astype(np.float32)),
    }


def dsa_inputs(inp, b, r, NSLOT=16):
    x = inp["x"][b]
    w_in = inp["ab_w_in"][0]
    tiles = dsa_slots(r)[:NSLOT]
    xq = np.concatenate([x[t * 128:(t + 1) * 128] for t in tiles], 0)
    q = np.arange(128)[:, None]; kk = np.arange(128)[None, :]
    tri = np.where(kk <= q, 0.0, NEG).astype(np.float32)
    cb = np.zeros((128, 2, 4, 128), np.float32)
    for hi, off in ((0, r), (1, 3 - r)):
        for j in range(4):
            cb[:, hi, j, :] = 0.0 if j < off else (tri if j == off else NEG)
    return {
        "xT": _C(x.T), "xqT": _C(xq.T),
        "wkv": _C(w_in[:, 256:448]),
        "wq": _C(np.concatenate([w_in[:, 0:256], w_in[:, 448:456]], 1)),
        "gq": _C(inp["ab_q_norm_g"][0][None]), "gkv": _C(inp["ab_kv_norm_g"][0][None]),
        "gki": _C(inp["ab_kidx_norm_g"][0][None]), "bki": _C(inp["ab_kidx_norm_b"][0][None]),
        "wuqT": _C(inp["ab_w_uq"][0].transpose(2, 1, 0)),
        "wukT": _C(inp["ab_w_uk"][0].transpose(2, 1, 0)),
        "wqidx": _C(inp["ab_w_qidx"][0].reshape(256, 512)),
        "wuv": _C(inp["ab_w_uv"][0].reshape(128, 512)),
        "cb": _C(cb.reshape(128, 2, 512)), "ident": np.eye(128, dtype=np.float32),
    }, tiles


def gdn_inputs(inp, b, h, S=8192):
    x = inp["x"][b][:S]
    w_in = inp["ab_w_in"][0]
    o = 456
    cols = lambda base: slice(base + 128 * h, base + 128 * (h + 1))
    wqkv = np.stack([w_in[:, cols(o)], w_in[:, cols(o + 512)], w_in[:, cols(o + 1024)]], 1)
    wz = np.concatenate([w_in[:, cols(o + 1536)], w_in[:, o + 2048 + h:o + 2049 + h], w_in[:, o + 2052 + h:o + 2053 + h]], 1)
    cwf = inp["ab_conv_w"][0]
    cw = np.stack([cwf[:, 512 * xx + 128 * h: 512 * xx + 128 * (h + 1)].T for xx in range(3)], 1)
    hp = np.broadcast_to(np.stack([inp["ab_a_log"][0][h], inp["ab_dt_bias"][0][h]]).astype(np.float32)[None], (128, 2))
    return {"xT": _C(x.T), "wqkv": _C(wqkv), "wz": _C(wz), "cw": _C(cw), "hp": _C(hp),
            "ng": _C(inp["ab_out_norm_g"][0][None]), "cst": gdn_consts()}


_NC_CACHE = {}


def _get(name, fn):
    if name not in _NC_CACHE:
        _NC_CACHE[name] = fn()
    return _NC_CACHE[name]


def kernel_unfused(**inputs):
    inp = {k: np.asarray(v, dtype=np.float32) for k, v in inputs.items()}
    B, S, D = 2, 8192, 1024
    x = inp["x"]
    maps, tls = [], []
    for c in range(NCORES):
        im, tiles = dsa_inputs(inp, c // 4, c % 4)
        maps.append(im); tls.append(tiles)
    outs = _run(build_dsa(), maps)
    o_a = np.zeros((B, S, 512), np.float32)
    for c in range(NCORES):
        for s_, t in enumerate(tls[c]):
            o_a[c // 4, t * 128:(t + 1) * 128] = outs[c][s_ * 128:(s_ + 1) * 128]
    del maps
    maps = [gdn_inputs(inp, c // 4, c % 4) for c in range(NCORES)]
    outs = _run(build_gdn(), maps)
    o_b = np.zeros((B, S, 512), np.float32)
    for c in range(NCORES):
        o_b[c // 4, :, 128 * (c % 4):128 * (c % 4 + 1)] = outs[c]
    del maps
    o_ab = np.concatenate([o_a, o_b], -1).reshape(B * S, D)
    xf = x.reshape(B * S, D)
    maps = []
    for c in range(NCORES):
        sl = slice(c * 2048, (c + 1) * 2048)
        maps.append({"oT": _C(o_ab[sl].T), "x": _C(xf[sl]), "w_out": _C(inp["ab_w_out"][0]),
                     "lng": _C(inp["ln_mix_g"][0][None]), "lnb": _C(inp["ln_mix_b"][0][None])})
    hm0 = np.concatenate(_run(build_mix(), maps), 0)
    del maps
    nc_moe = build_moe()
    pf = inp["p"].reshape(2, B * S, 256)
    base = moe_inputs(inp, 0, hm0[:1], pf[0, :1].T)
    maps = []
    for c in range(NCORES):
        sl = slice(c * 2048, (c + 1) * 2048)
        m = dict(base); m["h"] = _C(hm0[sl]); m["pT"] = _C(pf[0, sl].T)
        maps.append(m)
    h1 = np.concatenate(_run(nc_moe, maps), 0)
    del maps, base
    h1b = h1.reshape(B, S, D)
    maps = [cd_inputs(inp, h1b[c // 4], c % 4) for c in range(NCORES)]
    hm1 = np.concatenate(_run(build_cd(), maps), 0)
    del maps
    nc_moe = build_moe()
    base = moe_inputs(inp, 1, hm1[:1], pf[1, :1].T)
    maps = []
    for c in range(NCORES):
        sl = slice(c * 2048, (c + 1) * 2048)
        m = dict(base); m["h"] = _C(hm1[sl]); m["pT"] = _C(pf[1, sl].T)
        maps.append(m)
    out = np.concatenate(_run(nc_moe, maps), 0)
    return out.reshape(B, S, D).astype(np.float32)


def kernel(**inputs):
    inp = {k: np.asarray(v, dtype=np.float32) for k, v in inputs.items()}
    B, S, D = 2, 8192, 1024
    x = inp["x"]
    xf = x.reshape(B * S, D)
    pf = inp["p"].reshape(2, B * S, 256)
    nc = build_fused()
    ident = np.eye(128, dtype=np.float32)
    xTs = [_C(x[b].T) for b in range(B)]
    m0 = moe_inputs(inp, 0, xf[:1], pf[0, :1].T)
    m1 = moe_inputs(inp, 1, xf[:1], pf[1, :1].T)
    maps = []
    for c in range(NCORES):
        b, r = c // 4, c % 4
        sl = slice(c * 2048, (c + 1) * 2048)
        m = {"ident": ident, "xT": xTs[b]}
        da, _ = dsa_inputs(inp, b, r)
        for kk, v in da.items():
            if kk not in ("xT", "ident"):
                m["a_" + kk] = v
        dg = gdn_inputs(inp, b, r)
        for kk, v in dg.items():
            if kk != "xT":
                m["g_" + kk] = v
        sel = np.zeros((128, 8), np.float32); sel[:, c] = 1.0
        selp = np.zeros((128, 8), np.float32)
        if r > 0:
            selp[:, c - 1] = 1.0
        m["x_x"] = _C(xf[sl]); m["x_w_out"] = _C(inp["ab_w_out"][0])
        m["x_lng"] = _C(inp["ln_mix_g"][0][None]); m["x_lnb"] = _C(inp["ln_mix_b"][0][None]); m["x_sel"] = sel
        m["h_selp"] = selp
        for pfx, mm, L in (("m0_", m0, 0), ("m1_", m1, 1)):
            for kk, v in mm.items():
                if kk not in ("h", "ident", "pT"):
                    m[pfx + kk] = v
            m[pfx + "pT"] = _C(pf[L, sl].T)
        dc = cd_inputs(inp, np.zeros((S, D), np.float32), r)
        for kk, v in dc.items():
            if kk not in ("hx", "ident"):
                m["c_" + kk] = v
        maps.append(m)
    outs = _run(nc, maps)
    return np.concatenate(outs, 0).reshape(B, S, D).astype(np.float32)
```
